# Optimizing a Trainium2 kernel written in Bass

```python
import jax, jax.numpy as jnp
from jax import lax
import numpy as np

D_MODEL = 1024
BATCH = 16
SEQ = 2048
DEPTH = 2

CHUNK = 64
N_BRANCH = 4
BRANCH_W = D_MODEL // N_BRANCH
RMS_EPS = 1e-6
NEG_BIG = -1e30
LOG_FLOOR = 1e-30
HG_HEADS = 4
HG_DK = 128
HG_DV = BRANCH_W // HG_HEADS
POOL_WINDOWS = (2, 4, 8, 16)
POOL_GROUP = BRANCH_W // len(POOL_WINDOWS)
SA_HEADS = 4
SA_DH = BRANCH_W // SA_HEADS
IDX_HEADS = 8
IDX_DH = 64
TOPK_MAX = 256
Q_BLOCK = 128
N_MEM = 256
MEM_HEADS = 4
MEM_DH = BRANCH_W // MEM_HEADS
D_FF = -(-8 * D_MODEL // (3 * 256)) * 256
SPLIT_SIZES = (HG_HEADS * HG_DK, HG_HEADS * HG_DK, BRANCH_W, BRANCH_W,
               BRANCH_W,
               SA_HEADS * SA_DH, SA_DH, SA_DH,
               IDX_HEADS * IDX_DH, IDX_DH, IDX_HEADS,
               MEM_HEADS * MEM_DH)
N_IN = sum(SPLIT_SIZES)

kernel_name = "hybrid_hgrn2_pool_dsa_memxattn_block"

F32 = jnp.float32


def rms_norm(x, gain, eps=RMS_EPS):
    xf = x.astype(F32)
    y = xf * lax.rsqrt(jnp.mean(xf * xf, axis=-1, keepdims=True) + eps)
    return (y * gain.astype(F32)).astype(x.dtype)


def hgrn2_mixer(q, f_logit, i, g, lb, o_gain):
    B, T, _ = q.shape
    n = T // CHUNK
    lbf = lb.astype(F32)
    sg = jax.nn.sigmoid(f_logit.astype(F32))
    f = lbf + (1.0 - lbf) * sg
    log_f = jnp.log(jnp.maximum(f, LOG_FLOOR))
    k = (1.0 - lbf) * (1.0 - sg)
    qf = jax.nn.silu(q.astype(F32))

    def to_chunks(a, d):
        return a.reshape(B, n, CHUNK, HG_HEADS, d).transpose(1, 0, 2, 3, 4)

    xs = (to_chunks(qf, HG_DK), to_chunks(k, HG_DK), to_chunks(log_f, HG_DK),
          to_chunks(i.astype(F32), HG_DV))
    causal = jnp.tril(jnp.ones((CHUNK, CHUNK), dtype=bool))[None, :, :, None, None]

    def step(S, inp):
        qc, kc, lfc, vc = inp
        b = jnp.cumsum(lfc, axis=1)
        o_inter = jnp.einsum('bthk,bhkv->bthv', qc * jnp.exp(b), S)
        diff = jnp.where(causal, b[:, :, None] - b[:, None, :], 0.0)
        decay = jnp.where(causal, jnp.exp(diff), 0.0)
        att = jnp.einsum('bthk,bshk,btshk->bhts', qc, kc, decay)
        o_intra = jnp.einsum('bhts,bshv->bthv', att, vc)
        b_last = b[:, -1]
        S = jnp.exp(b_last)[..., None] * S + jnp.einsum(
            'bshk,bshv->bhkv', kc * jnp.exp(b_last[:, None] - b), vc)
        return S, o_inter + o_intra

    S0 = jnp.zeros((B, HG_HEADS, HG_DK, HG_DV), F32)
    _, o = lax.scan(step, S0, xs)
    o = o.transpose(1, 0, 2, 3, 4).reshape(B, T, HG_HEADS, HG_DV)
    o = o * lax.rsqrt(jnp.mean(o * o, axis=-1, keepdims=True) + RMS_EPS)
    o = o.reshape(B, T, BRANCH_W) * o_gain.astype(F32) * jax.nn.silu(g.astype(F32))
    return o.astype(q.dtype)


def pool_mixer(z, pool_w, pool_scale):
    B, T, _ = z.shape
    zf = z.astype(F32).reshape(B, T, len(POOL_WINDOWS), POOL_GROUP)
    csum = jnp.cumsum(zf, axis=1)
    pos = jnp.arange(T)
    means = []
    for gi, w in enumerate(POOL_WINDOWS):
        c = csum[:, :, gi]
        lagged = jnp.pad(c, ((0, 0), (w, 0), (0, 0)))[:, :T]
        cnt = jnp.minimum(pos + 1, w).astype(F32)
        means.append((c - lagged) / cnt[None, :, None])
    pooled = jnp.stack(means, axis=2) - zf
    y = jnp.einsum('btgc,gcd->btgd', pooled, pool_w.astype(F32)).reshape(B, T, BRANCH_W)
    return (y * pool_scale.astype(F32)).astype(z.dtype)


def dsa_mixer(q, k, v, iq, ik, iw):
    B, T, _ = q.shape
    n_sel = min(TOPK_MAX, T // 4)
    nb = T // Q_BLOCK

    def blocks(a, tail):
        return jnp.moveaxis(a.reshape((B, nb, Q_BLOCK) + tail), 1, 0)

    qb_all = blocks(q, (SA_HEADS, SA_DH))
    iqb_all = blocks(iq, (IDX_HEADS, IDX_DH))
    iwb_all = blocks(iw, (IDX_HEADS,))
    key_pos = jnp.arange(T)
    gather = jax.vmap(lambda arr, idx: arr[idx])

    def block(args):
        bi, qb, iqb, iwb = args
        qpos = bi * Q_BLOCK + jnp.arange(Q_BLOCK)
        limit = (qpos // CHUNK + 1) * CHUNK
        admissible = key_pos[None, :] < limit[:, None]
        logits = jnp.einsum('bqhd,bsd->bqsh', iqb, ik).astype(F32) * (IDX_DH ** -0.5)
        score = jnp.einsum('bqsh,bqh->bqs', jax.nn.relu(logits),
                           iwb.astype(F32) * (IDX_HEADS ** -0.5))
        score = jnp.where(admissible[None], score, NEG_BIG)
        top_val, top_idx = lax.top_k(score, n_sel)
        valid = top_val > 0.5 * NEG_BIG
        k_sel = gather(k, top_idx)
        v_sel = gather(v, top_idx)
        s = jnp.einsum('bqhd,bqkd->bhqk', qb, k_sel).astype(F32) * (SA_DH ** -0.5)
        s = jnp.where(valid[:, None], s, NEG_BIG)
        p = jax.nn.softmax(s, axis=-1)
        o = jnp.einsum('bhqk,bqkd->bqhd', p.astype(v.dtype), v_sel)
        return o.reshape(B, Q_BLOCK, SA_HEADS * SA_DH)

    out = lax.map(block, (jnp.arange(nb), qb_all, iqb_all, iwb_all))
    return jnp.moveaxis(out, 0, 1).reshape(B, T, SA_HEADS * SA_DH)


def mem_attention(qm, mk, mv):
    B, T, _ = qm.shape
    qh = qm.reshape(B, T, MEM_HEADS, MEM_DH)
    kh = mk.reshape(B, N_MEM, MEM_HEADS, MEM_DH)
    vh = mv.reshape(B, N_MEM, MEM_HEADS, MEM_DH)
    s = jnp.einsum('bthd,bnhd->bhtn', qh, kh).astype(F32) * (MEM_DH ** -0.5)
    p = jax.nn.softmax(s, axis=-1)
    o = jnp.einsum('bhtn,bnhd->bthd', p.astype(vh.dtype), vh)
    return o.reshape(B, T, MEM_HEADS * MEM_DH)


def setup_inputs(seed: int = 0) -> dict:
    key = jax.random.key(seed)
    ks = jax.random.split(key, 18)
    L, D = DEPTH, D_MODEL
    nrm = lambda k, shape, fan: jax.random.normal(k, shape, F32) * (fan ** -0.5)
    gain = lambda k, shape: 1.0 + 0.1 * jax.random.normal(k, shape, F32)
    return {
        "x": jax.random.normal(ks[0], (BATCH, SEQ, D), F32),
        "mem": jax.random.normal(ks[1], (BATCH, N_MEM, D), F32),
        "lb_logits": jax.random.normal(ks[2], (L, HG_HEADS * HG_DK), F32),
        "w_in": nrm(ks[3], (L, D, N_IN), D),
        "w_gate": nrm(ks[4], (L, N_BRANCH, D, D), D),
        "w_branch": nrm(ks[5], (L, N_BRANCH, BRANCH_W, D), BRANCH_W),
        "w_out": nrm(ks[6], (L, D, D), D),
        "hgrn_norm": gain(ks[7], (L, BRANCH_W)),
        "pool_w": nrm(ks[8], (L, len(POOL_WINDOWS), POOL_GROUP, POOL_GROUP), POOL_GROUP),
        "pool_scale": gain(ks[9], (L, BRANCH_W)),
        "w_mem_kv": nrm(ks[10], (L, D, 2 * MEM_HEADS * MEM_DH), D),
        "norm_mem": gain(ks[11], (L, D)),
        "norm_mix_pre": gain(ks[12], (L, D)),
        "norm_mix_post": gain(ks[13], (L, D)),
        "norm_ffn_pre": gain(ks[14], (L, D)),
        "norm_ffn_post": gain(ks[15], (L, D)),
        "w_ffn_gate_up": nrm(ks[16], (L, D, 2 * D_FF), D),
        "w_ffn_down": nrm(ks[17], (L, D_FF, D), D_FF),
    }


def reference(x, mem, lb_logits, w_in, w_gate, w_branch, w_out, hgrn_norm, pool_w,
              pool_scale, w_mem_kv, norm_mem, norm_mix_pre, norm_mix_post,
              norm_ffn_pre, norm_ffn_post, w_ffn_gate_up, w_ffn_down):
    split_pts = [int(p) for p in np.cumsum(SPLIT_SIZES)[:-1]]
    lb_soft = jax.nn.softmax(lb_logits.astype(F32), axis=0)
    lb_all = jnp.cumsum(lb_soft, axis=0) - lb_soft[0:1]
    h = x
    for l in range(DEPTH):
        u = rms_norm(h, norm_mix_pre[l])
        z = u @ w_in[l]
        (hq, hf, hi, hg, pz, sq, sk, sv, iq, ik, iw, mq) = jnp.split(z, split_pts, axis=-1)
        y_a = hgrn2_mixer(hq, hf, hi, hg, lb_all[l], hgrn_norm[l])
        y_b = pool_mixer(pz, pool_w[l], pool_scale[l])
        y_c = dsa_mixer(sq, sk, sv, iq, ik, iw)
        mkv = rms_norm(mem, norm_mem[l]) @ w_mem_kv[l]
        mk, mv = jnp.split(mkv, 2, axis=-1)
        y_m = mem_attention(mq, mk, mv)
        merged = None
        for bi, y in enumerate((y_a, y_b, y_c, y_m)):
            term = jax.nn.sigmoid(u @ w_gate[l, bi]) * (y @ w_branch[l, bi])
            merged = term if merged is None else merged + term
        h = h + rms_norm(merged @ w_out[l], norm_mix_post[l])
        u = rms_norm(h, norm_ffn_pre[l])
        gt, up = jnp.split(u @ w_ffn_gate_up[l], 2, axis=-1)
        h = h + rms_norm((jax.nn.silu(gt) * up) @ w_ffn_down[l], norm_ffn_post[l])
    return h
```

```python
import numpy as np
import concourse.bass as bass
import concourse.mybir as mybir
from concourse.bass_utils import run_bass_kernel_spmd

F32 = mybir.dt.float32
BF16 = mybir.dt.bfloat16
AF = mybir.ActivationFunctionType
ALU = mybir.AluOpType
AX = mybir.AxisListType

D = 1024
TB = 512
DFF = 2816
NIN = 3016
NMEM = 256
EPS = 1e-6
PAGE = 64
NBIS = 16
DVE_INORDER = False


class View:
    __slots__ = ("ap", "keys")

    def __init__(self, ap, keys):
        self.ap = ap
        self.keys = keys


class Buf:
    def __init__(self, nc, name, shape, dtype, off):
        self.shape = list(shape)
        self.esz = 4 if dtype == F32 else 2
        self.off = off
        self.h = nc.alloc_sbuf_tensor_at(name, self.shape, dtype, offset=off)
        self.strides = []
        s = 1
        for d in reversed(self.shape[1:]):
            self.strides.insert(0, s)
            s *= d
        self.nelem = s
        self.nbytes = s * self.esz

    def __getitem__(self, idx):
        if not isinstance(idx, tuple):
            idx = (idx,)
        lo = 0
        hi = 0
        for k, st in enumerate(self.strides):
            dim = self.shape[k + 1]
            if k + 1 < len(idx):
                i = idx[k + 1]
                if isinstance(i, slice):
                    a = 0 if i.start is None else i.start
                    b = dim if i.stop is None else i.stop
                else:
                    a, b = i, i + 1
            else:
                a, b = 0, dim
            lo += a * st
            hi += (b - 1) * st
        hi += 1
        b0 = (self.off + lo * self.esz) // PAGE
        b1 = (self.off + hi * self.esz - 1) // PAGE
        return View(self.h[idx], [("sb", p) for p in range(b0, b1 + 1)])

    def all(self):
        return self[(slice(None),) * len(self.shape)]


class Prog:
    ENG = ("sp", "pe", "act", "dve", "pool")

    def __init__(self, nc):
        self.nc = nc
        self.ops = {e: [] for e in self.ENG}
        self.cnt = {}
        self.sem = {}
        self.res = {}
        self.known = {e: {} for e in self.ENG}
        self.epoch = {}
        self.nops = 0
        self.tag = ''
        self.tags = {e: [] for e in self.ENG}

    def _sem(self, key):
        if key not in self.sem:
            self.sem[key] = self.nc.alloc_semaphore("s_" + key)
            self.cnt[key] = 0

    def add(self, eng, fn, reads=(), writes=(), dma=None):
        deps = set()
        rk = []
        wk = []
        for v in reads:
            for key in (v.keys if isinstance(v, View) else [v]):
                if isinstance(key, tuple) and key[0] == "ps":
                    wk.append(key)
                else:
                    rk.append(key)
        for v in writes:
            wk.extend(v.keys if isinstance(v, View) else [v])
        for k in rk:
            r = self.res.get(k)
            if r is not None and r[0] is not None:
                deps.add(r[0])
        for k in wk:
            r = self.res.get(k)
            if r is not None:
                if r[0] is not None:
                    deps.add(r[0])
                deps.update(r[1])
        if dma:
            semkey = dma
        else:
            ep = self.epoch.get(eng, 0)
            semkey = "%s_%d" % (eng, ep)
            if self.cnt.get(semkey, 0) >= 4000:
                self.epoch[eng] = ep + 1
                semkey = "%s_%d" % (eng, ep + 1)
        self._sem(semkey)
        inc = 16 if dma else 1
        self.cnt[semkey] += inc
        tok = (semkey, self.cnt[semkey])
        waits = {}
        kn = self.known[eng]
        for (sk, v) in deps:
            if eng == "pe" and sk.startswith("pe_"):
                continue
            if eng == "dve" and sk.startswith("dve_") and DVE_INORDER:
                continue
            if kn.get(sk, 0) >= v:
                continue
            if waits.get(sk, 0) < v:
                waits[sk] = v
        for sk, v in waits.items():
            kn[sk] = v
        self.ops[eng].append((list(waits.items()), fn, semkey, inc))
        self.tags[eng].append(self.tag)
        self.nops += 1
        for k in rk:
            r = self.res.get(k)
            if r is None:
                self.res[k] = [None, [tok]]
            else:
                r[1].append(tok)
        for k in wk:
            self.res[k] = [tok, []]
        return tok

    def emit(self):
        nc = self.nc

        def mk(eng):
            def body(e):
                for waits, fn, semkey, inc in self.ops[eng]:
                    for sk, v in waits:
                        e.wait_ge(self.sem[sk], v)
                    fn(e).then_inc(self.sem[semkey], inc)
            return body

        with nc.Block() as block:
            block.sync(mk("sp"))
            block.tensor(mk("pe"))
            block.scalar(mk("act"))
            block.vector(mk("dve"))
            block.gpsimd(mk("pool"))


def _a(v):
    return v.ap if isinstance(v, View) else v


def _vs(*xs):
    return [x for x in xs if isinstance(x, View)]


class K:
    def __init__(self, P):
        self.P = P

    def mm(self, out, lhsT, rhs, start=True, stop=True):
        self.P.add("pe", lambda e: e.matmul(out.ap, lhsT.ap, rhs.ap, start=start, stop=stop),
                   reads=[lhsT, rhs], writes=[out])

    def tr(self, out, in_, ident):
        self.P.add("pe", lambda e: e.transpose(out.ap, in_.ap, ident.ap), reads=[in_, ident], writes=[out])

    def act(self, out, in_, func, bias=None, scale=None, accum=None):
        kw = {}
        if bias is not None:
            kw["bias"] = _a(bias)
        if scale is not None:
            kw["scale"] = _a(scale)
        if accum is not None:
            kw["accum_out"] = accum.ap
        self.P.add("act", lambda e: e.activation(out.ap, in_.ap, func, **kw),
                   reads=_vs(in_, bias, scale), writes=_vs(out, accum))

    def ts(self, eng, out, in0, s1, s2, op0, op1=None, accum=None):
        kw = {}
        if op1 is not None:
            kw["op1"] = op1
        if accum is not None:
            kw["accum_out"] = accum.ap
        self.P.add(eng, lambda e: e.tensor_scalar(out.ap, in0.ap, _a(s1), _a(s2), op0, **kw),
                   reads=_vs(in0, s1, s2), writes=_vs(out, accum))

    def tt(self, eng, out, in0, in1, op):
        self.P.add(eng, lambda e: e.tensor_tensor(out.ap, in0.ap, in1.ap, op), reads=[in0, in1], writes=[out])

    def stt(self, out, in0, s, in1, op0, op1):
        self.P.add("dve", lambda e: e.scalar_tensor_tensor(out.ap, in0.ap, _a(s), in1.ap, op0, op1),
                   reads=_vs(in0, s, in1), writes=[out])

    def cp(self, eng, out, in_):
        if eng == "act":
            self.P.add("act", lambda e: e.copy(out.ap, in_.ap), reads=[in_], writes=[out])
        else:
            self.P.add(eng, lambda e: e.tensor_copy(out.ap, in_.ap), reads=[in_], writes=[out])

    def recip(self, out, in_):
        self.P.add("dve", lambda e: e.reciprocal(out.ap, in_.ap), reads=[in_], writes=[out])

    def memset(self, eng, out, val):
        self.P.add(eng, lambda e: e.memset(out.ap, val), writes=[out])

    def reduce(self, out, in_, op, axis=AX.X, absval=None):
        self.P.add("dve", lambda e: e.tensor_reduce(out.ap, in_.ap, axis, op, apply_absolute_value=absval),
                   reads=[in_], writes=[out])

    def scan(self, out, d0, d1, init, op0, op1):
        self.P.add("dve", lambda e: e.tensor_tensor_scan(out.ap, d0.ap, d1.ap, _a(init), op0, op1),
                   reads=_vs(d0, d1, init), writes=[out])

    def dma(self, q, out, in_, sem, reads=(), writes=()):
        self.P.add(q, lambda e: e.dma_start(out=_a(out), in_=_a(in_)),
                   reads=list(reads) + _vs(in_), writes=list(writes) + _vs(out), dma=sem)


C_OFF = {}


def _mk_consts():
    cols = []

    def put(name, arr):
        C_OFF[name] = sum(a.shape[1] for a in cols)
        cols.append(arr.astype(np.float32))

    p = np.arange(128)
    put("ident", np.eye(128))
    put("ones", np.ones((128, 128)))
    bd = np.zeros((128, 128))
    bd[:64, :64] = 1
    bd[64:, 64:] = 1
    put("bdones", bd)
    t = np.arange(512)
    put("scanmask", np.tile((t % 64 != 0).astype(np.float32)[None, :], (128, 1)))
    s_ = p[:, None]
    t_ = np.arange(128)[None, :]
    tri = ((s_ // 64 == t_ // 64) & (t_ >= s_)).astype(np.float32)
    put("trimask", np.tile(tri, (1, 4)))
    inad = (p[:, None] < 64) & (np.arange(128)[None, :] >= 64)
    put("negadm", np.where(inad, -1e30, 0.0))
    put("mbdiag", np.where(inad, -30000.0, 0.0))
    ic = np.zeros((128, 2, 16))
    ws = [[2, 4], [8, 16]]
    for ci in range(2):
        for half in range(2):
            w = ws[ci][half]
            ic[half * 64:(half + 1) * 64, ci, :] = 1.0 / np.minimum(np.arange(16) + 1, w)[None, :]
    put("invcnt", ic.reshape(128, 32))
    iw = np.zeros((128, 2))
    for ci in range(2):
        for half in range(2):
            iw[half * 64:(half + 1) * 64, ci] = 1.0 / ws[ci][half]
    put("invw", iw)
    put("eps", np.full((128, 1), EPS))
    put("c256", np.full((128, 1), 256.0))
    put("pow2", np.tile((2.0 ** -np.arange(24))[None, :], (128, 1)))
    return np.ascontiguousarray(np.concatenate(cols, axis=1), dtype=np.float32)


CONSTS = _mk_consts()
NCONST = CONSTS.shape[1]
PV_PER_L = 44


def _mk_pvec(inp, L):
    cols = []
    for l in range(L):
        for nm in ("norm_mix_pre", "norm_mix_post", "norm_ffn_pre", "norm_ffn_post", "norm_mem"):
            cols.append(np.asarray(inp[nm][l]).reshape(8, 128).T)
        cols.append(np.asarray(inp["hgrn_norm"][l]).reshape(2, 128).T)
        cols.append(np.asarray(inp["pool_scale"][l]).reshape(2, 128).T)
    for l in range(L):
        cols.append(np.asarray(inp["lb_logits"][l]).reshape(4, 128).T)
    return np.ascontiguousarray(np.concatenate(cols, axis=1), dtype=np.float32)


def _mk_poolw(pw, L):
    out = np.zeros((L, 128, 2, 128), np.float32)
    for l in range(L):
        for g in range(4):
            ci, half = g // 2, g % 2
            out[l, half * 64:(half + 1) * 64, ci, half * 64:(half + 1) * 64] = pw[l, g]
    return out


def build(nseq, T, L=2, en=("a", "b", "c", "m")):
    nc = bass.Bass("TRN2", target_bir_lowering=False)
    NBLK = T // TB
    NT = T // 128
    P = Prog(nc)
    k = K(P)

    def din(name, shape):
        return nc.dram_tensor(name, list(shape), F32, kind="ExternalInput").ap()

    x_d = din("x", [nseq, T, D])
    mem_d = din("mem", [nseq, NMEM, D])
    consts_d = din("consts", [128, NCONST])
    NPV = PV_PER_L * L + 4 * L
    pvec_d = din("pvec", [128, NPV])
    poolw_d = din("poolw", [L, 128, 2, 128])
    w_in_d = din("w_in", [L, D, NIN])
    w_gate_d = din("w_gate", [L, 4, D, D])
    w_branch_d = din("w_branch", [L, 4, 256, D])
    w_out_d = din("w_out", [L, D, D])
    w_mem_d = din("w_mem_kv", [L, D, 512])
    w_gu_d = din("w_ffn_gate_up", [L, D, 2 * DFF])
    w_dn_d = din("w_ffn_down", [L, DFF, D])
    out_d = nc.dram_tensor("out", [nseq, T, D], F32, kind="ExternalOutput").ap()

    off = [((nc.sbuf_base + 63) // 64) * 64]
    sb_top = nc.sbuf_top

    def alloc(name, shape, dtype, at=None):
        esz = 4 if dtype == F32 else 2
        n = 1
        for d_ in shape[1:]:
            n *= d_
        nb = ((n * esz + 63) // 64) * 64
        if at is None:
            o = off[0]
            off[0] += nb
        else:
            o = at
        assert o + nb <= sb_top, (name, o, nb, sb_top)
        return Buf(nc, name, shape, dtype, o)

    cst = alloc("cst", [128, NCONST], F32)
    pv = alloc("pv", [128, NPV], F32)
    lbt = alloc("lbt", [128, L, 3, 4], F32)
    ident_b = alloc("ident_b", [128, 128], BF16)
    ones_b = alloc("ones_b", [128, 128], BF16)
    bdones_b = alloc("bdones_b", [128, 128], BF16)
    poolw_b = alloc("poolw_b", [128, L, 2, 128], BF16)
    hT = alloc("hT", [128, 8, TB], F32)
    uT = alloc("uT", [128, 8, TB], BF16)
    rstd = alloc("rstd", [128, TB], F32)
    bigf = alloc("bigf", [128, 8, TB], F32)
    xt = Buf(nc, "xt", [128, 4, D], F32, bigf.off)
    merged_b = alloc("merged_b", [128, 8, TB], BF16)
    ring = [alloc("ring%d" % i, [128, 8, 512], BF16) for i in range(4)]
    wbr = alloc("wbr", [128, 8, 512], BF16)
    yT = {b_: alloc("yT_" + b_, [128, 2, TB], BF16) for b_ in "abcm"}
    ikdup = [alloc("ikdup%d" % l, [128, T], BF16) for l in range(L)]
    skdup = [alloc("skdup%d" % l, [128, T], BF16) for l in range(L)]
    vdup = [alloc("vdup%d" % l, [128, NT, 128], BF16) for l in range(L)]
    Sst = alloc("Sst", [128, L, 4, 64], F32)
    halo = alloc("halo", [128, L, 2, 16], F32)
    mk2T = [alloc("mk2T%d" % l, [128, 2, NMEM], BF16) for l in range(L)]
    mvdup = [alloc("mvdup%d" % l, [128, 2, 4, 128], BF16) for l in range(L)]
    sqr = [alloc("sqr%d" % i, [128, TB], BF16) for i in range(2)]
    tmpn = alloc("tmpn", [128, TB], F32)
    zbuf = alloc("zbuf", [128, 2, 16 + TB], F32)
    sqpad = alloc("sqpad", [128, 4, TB], BF16)
    iqpad = alloc("iqpad", [128, 8, TB], BF16)
    mqpad = alloc("mqpad", [128, 4, TB], BF16)
    absw = alloc("absw", [128, 4, 8], F32)
    sgn = alloc("sgn", [128, 4, 8], F32)
    ipad = alloc("ipad", [128, 4, 4, 128], BF16)
    cols = alloc("cols", [128, 2, 32], F32)
    Spad = [alloc("Spad%d" % i, [128, 8, 128], BF16) for i in range(2)]
    ktok = alloc("ktok", [128, 4, 2, 128], BF16)
    arena0 = off[0]

    def phase():
        off[0] = arena0

    phase()
    itok = alloc("itok", [128, 4, 256], BF16)
    gs = alloc("gs", [128, 2, TB], BF16)
    hA = alloc("hA", [128, TB], F32)
    hB = alloc("hB", [128, TB], F32)
    hC = alloc("hC", [128, TB], F32)
    hQ = alloc("hQ", [128, TB], F32)
    hE = alloc("hE", [128, TB], F32)
    hF = alloc("hF", [128, TB], F32)
    hA2 = alloc("hA2", [128, TB], F32)
    hE2 = alloc("hE2", [128, TB], F32)
    Qp = alloc("Qp", [128, TB], BF16)
    Kp = alloc("Kp", [128, TB], BF16)
    qS = [alloc("qS%d" % i, [128, TB], BF16) for i in range(2)]
    attm = [alloc("attm%d" % i, [128, TB], BF16) for i in range(2)]
    dcol = alloc("dcol", [128, 8], F32)
    oT = alloc("oT", [128, TB], F32)
    end_h = off[0]
    phase()
    s2b = alloc("s2b", [128, 16 + TB], F32)
    s4b = alloc("s4b", [128, 16 + TB], F32)
    s8b = alloc("s8b", [128, 16 + TB], F32)
    sfin = alloc("sfin", [128, 16 + TB], F32)
    pooled_b = alloc("pooled_b", [128, 2, TB], BF16)
    end_p = off[0]
    phase()
    Dh = alloc("Dh", [128, 8, 128], BF16)
    rh = [alloc("rh%d" % i, [128, 512], BF16) for i in range(3)]
    score = [alloc("score%d" % i, [128, T], F32) for i in range(2)]
    mbias = [alloc("mbias%d" % i, [128, T], BF16) for i in range(2)]
    pT = [alloc("pT%d" % i, [128, 512], BF16) for i in range(2)]
    rec = alloc("rec", [128, 512], F32)
    end_d = off[0]
    phase()
    memT = alloc("memT", [128, 8, NMEM], F32)
    unT = alloc("unT", [128, 8, NMEM], BF16)
    end_m = off[0]
    phase()
    sig = [alloc("sig%d" % i, [128, TB], BF16) for i in range(2)]
    tb = [alloc("tb%d" % i, [128, TB], F32) for i in range(2)]
    end_g = off[0]
    phase()
    actT = alloc("actT", [128, 22, TB], BF16)
    sgt = [alloc("sgt%d" % i, [128, TB], BF16) for i in range(2)]
    end_f = off[0]
    print('SBUF use', arena0, max(end_h, end_p, end_d, end_m, end_g, end_f), sb_top)
    assert max(end_h, end_p, end_d, end_m, end_g, end_f) <= sb_top

    psb = [nc.alloc_psum_tensor("ps%d" % i, [128, 512], F32) for i in range(8)]
    rr = [0]

    def ps(bank=None):
        if bank is None:
            bank = rr[0] % 6
            rr[0] += 1
        return bank

    def pv_(bank, *idx):
        if not idx:
            idx = (slice(None), slice(None))
        return View(psb[bank][idx], [("ps", bank)])

    def c_(name, n, rows=slice(None)):
        o = C_OFF[name]
        return cst[rows, o:o + n]

    ident_f = c_("ident", 128)
    eps_c = c_("eps", 1)

    def pvc(l, grp, j):
        o = PV_PER_L * l + grp * 8 + j
        return pv[:, o:o + 1]

    def pv_hgn(l, j):
        o = PV_PER_L * l + 40 + j
        return pv[:, o:o + 1]

    def pv_psc(l, j):
        o = PV_PER_L * l + 42 + j
        return pv[:, o:o + 1]

    scr = {}

    def mk_scr(name, nk, ncols):
        scr[name] = nc.dram_tensor("scr_" + name, [128, nk, ncols], BF16, kind="Internal").ap()

    cast_i = [0]

    def cast(name, c0, src, sem):
        import os
        if os.environ.get("KNOCAST"):
            return
        n = src.shape[-1]
        cast_i[0] += 1
        dst = scr[name][:, 0:src.shape[1], c0:c0 + n]
        P.add("pool", lambda e: e.dma_start(out=dst, in_=src), reads=[], writes=["cast%d" % cast_i[0]], dma=sem)

    def finish_cast(names, sem):
        if sem not in P.cnt:
            return
        tok = (sem, P.cnt[sem])
        for nm in names:
            P.res[nm] = [tok, []]

    def rows_kc(ap2d):
        return ap2d.rearrange("(kc p) n -> p kc n", p=128)

    def sync_group(views, sem):
        tok = (sem, P.cnt[sem])
        for v in views:
            for key in v.keys:
                P.res[key] = [tok, []]

    P.tag = 'setup'
    k.dma("sp", cst.all(), consts_d, "init")
    k.dma("sp", pv.all(), pvec_d, "init")
    sync_group([cst.all(), pv.all()], "init")
    k.cp("dve", ident_b.all(), ident_f)
    k.cp("dve", ones_b.all(), c_("ones", 128))
    k.cp("dve", bdones_b.all(), c_("bdones", 128))
    for buf in (sqpad, iqpad, mqpad, ipad, ktok, Spad[0], Spad[1]):
        k.memset("pool", buf.all(), 0.0)
    lo_ = PV_PER_L * L
    for l in range(L):
        if l == 0:
            k.memset("dve", lbt[:, l, 0, :], 0.0)
        else:
            assert L == 2
            k.tt("dve", lbt[:, l, 1, :], pv[:, lo_ + 4:lo_ + 8], pv[:, lo_:lo_ + 4], ALU.subtract)
            k.act(lbt[:, l, 0, :], lbt[:, l, 1, :], AF.Sigmoid)
        k.ts("dve", lbt[:, l, 1, :], lbt[:, l, 0, :], -1.0, 1.0, ALU.mult, ALU.add)
        k.ts("dve", lbt[:, l, 2, :], lbt[:, l, 1, :], -1.0, None, ALU.mult)
    for l in range(L):
        k.dma("sp", tmpn[:, 0:256], poolw_d[l].rearrange("p a b -> p (a b)"), "init2")
        k.cp("dve", poolw_b[:, l, :, :], View(tmpn.h[:, 0:256].rearrange("p (a b) -> p a b", b=128), tmpn[:, 0:256].keys))

    stage = [Buf(nc, "stage0", [128, 8, 512], F32, bigf.off), Buf(nc, "stage1", [128, 8, 512], F32, hT.off)]
    cast_n = [0]

    def cast_unit(nm, nk, ncols, pieces):
        mk_scr(nm, nk, ncols)
        i = cast_n[0]
        cast_n[0] += 1
        stg = stage[i % 2]
        for (k0, c0, s_ap) in pieces:
            nkk, n = s_ap.shape[1], s_ap.shape[2]
            k.dma("sp", stg[:, k0:k0 + nkk, c0:c0 + n], s_ap, "cin%d" % (i % 2))
        sync_group([stg.all()], "cin%d" % (i % 2))
        rb = ring[i % 4][:, 0:nk, 0:ncols]
        k.cp(("dve", "act", "pool")[i % 3], rb, stg[:, 0:nk, 0:ncols])
        dst = scr[nm]
        P.add("sp", lambda e: e.dma_start(out=dst, in_=rb.ap), reads=[rb], writes=[nm], dma="cout%d" % (i % 4))

    for l in range(L):
        wi = rows_kc(w_in_d[l])
        cast_unit("inT_%d" % l, 8, 328, [(0, 0, wi[:, :, 1024:1280]), (0, 256, wi[:, :, 2112:2176]),
                                         (0, 320, wi[:, :, 2752:2760])])
        cast_unit("in2_%d" % l, 8, 512, [(0, 0, wi[:, :, 1280:1792])])
        cast_unit("in0_%d" % l, 8, 512, [(0, 0, wi[:, :, 0:512])])
        cast_unit("in1_%d" % l, 8, 512, [(0, 0, wi[:, :, 512:1024])])
        cast_unit("in3_%d" % l, 8, 512, [(0, 0, wi[:, :, 1792:2048]), (0, 256, wi[:, :, 2048:2112]),
                                         (0, 320, wi[:, :, 2048:2112]), (0, 384, wi[:, :, 2688:2752]),
                                         (0, 448, wi[:, :, 2688:2752])])
        cast_unit("in4_%d" % l, 8, 512, [(0, 0, wi[:, :, 2176:2688])])
        cast_unit("in5_%d" % l, 8, 256, [(0, 0, wi[:, :, 2760:3016])])
        cast_unit("mem_%d" % l, 8, 512, [(0, 0, rows_kc(w_mem_d[l]))])
        for half in range(2):
            cast_unit("br%d_%d" % (half, l), 8, 512,
                      [(2 * b_, 0, rows_kc(w_branch_d[l, b_])[:, :, 512 * half:512 * half + 512]) for b_ in range(4)])
        for half in range(2):
            for b_ in range(4):
                cast_unit("g%d_%d_%d" % (b_, half, l), 8, 512,
                          [(0, 0, rows_kc(w_gate_d[l, b_])[:, :, 512 * half:512 * half + 512])])
        for half in range(2):
            cast_unit("o%d_%d" % (half, l), 8, 512, [(0, 0, rows_kc(w_out_d[l])[:, :, 512 * half:512 * half + 512])])
        wgu = rows_kc(w_gu_d[l])
        for i in range(11):
            cast_unit("gu%d_%d" % (i, l), 8, 512, [(0, 0, wgu[:, :, 256 * i:256 * i + 256]),
                                                  (0, 256, wgu[:, :, DFF + 256 * i:DFF + 256 * i + 256])])
        wdn = w_dn_d[l].rearrange("(j p) n -> p j n", p=128)
        for half in range(2):
            for jg in range(3):
                nj = min(8, 22 - 8 * jg)
                cast_unit("d%d_%d_%d" % (jg, half, l), nj, 512,
                          [(0, 0, wdn[:, 8 * jg:8 * jg + nj, 512 * half:512 * half + 512])])

    rslot = [0]

    def load_unit(name, nk=8, ncols=512):
        s_ = rslot[0] % 4
        rslot[0] += 1
        dst = ring[s_][:, 0:nk, 0:ncols]
        src = scr[name][:, 0:nk, 0:ncols]
        import os
        if os.environ.get("KNOLOAD") and name.startswith("d"):
            return ring[s_]
        P.add("sp", lambda e: e.dma_start(out=dst.ap, in_=src), reads=[name], writes=[dst], dma="r%d" % s_)
        return ring[s_]

    def finish_rstd(bank, ncols, div, dst):
        k.act(tmpn[:, 0:ncols], pv_(bank, slice(None), slice(0, ncols)), AF.Sqrt, bias=eps_c, scale=1.0 / div)
        k.recip(dst, tmpn[:, 0:ncols])

    def prenorm(src, dst_bf, gain_grp, l, ncols, rst):
        for c in range(8):
            if c % 2 == 0:
                k.act(dst_bf[:, c, 0:ncols], src[:, c, 0:ncols], AF.Square)
            else:
                k.tt("dve", dst_bf[:, c, 0:ncols], src[:, c, 0:ncols], src[:, c, 0:ncols], ALU.mult)
        for c in range(8):
            k.mm(pv_(7, slice(None), slice(0, ncols)), ones_b.all(), dst_bf[:, c, 0:ncols], start=(c == 0), stop=(c == 7))
        finish_rstd(7, ncols, 1024.0, rst)
        for c in range(8):
            k.stt(dst_bf[:, c, 0:ncols], src[:, c, 0:ncols], pvc(l, gain_grp, c), rst, ALU.mult, ALU.mult)

    def post_evac(bank, fc, l, grp):
        import os
        ne = os.environ.get("KNOEVAC", "")
        if "d" not in ne:
            k.ts("dve", bigf[:, fc, :], pv_(bank), pvc(l, grp, fc), None, ALU.mult)
        if "a" not in ne:
            k.act(uT[:, fc, :], pv_(bank), AF.Square)

    def post_finish():
        for fc in range(8):
            k.mm(pv_(7), ones_b.all(), uT[:, fc, :], start=(fc == 0), stop=(fc == 7))
        finish_rstd(7, TB, 1024.0, rstd.all())
        for fc in range(8):
            k.tt("pool", bigf[:, fc, :], bigf[:, fc, :], rstd.all(), ALU.mult)
            k.tt("dve", hT[:, fc, :], hT[:, fc, :], bigf[:, fc, :], ALU.add)

    def fm_chunk(wbuf, col0, bank=None, rhs=None, ncols=TB):
        b_ = ps(bank)
        for kc in range(8):
            k.mm(pv_(b_, slice(None), slice(0, ncols)), wbuf[:, kc, col0:col0 + 128],
                 (rhs if rhs is not None else uT)[:, kc, 0:ncols], start=(kc == 0), stop=(kc == 7))
        return b_


    for b_ in "abcm":
        if b_ not in en:
            k.memset("pool", yT[b_].all(), 0.0)
    S0 = slice(None)
    CI = (IDX_C := (64 ** -0.5) * (8 ** -0.5))

    def mem_setup(s):
        P.tag = 'memsetup'
        for nb in range(2):
            k.dma("sp", xt[:, nb, :], mem_d[s, 128 * nb:128 * nb + 128, :], "xin")
        sync_group([xt[:, 0:2, :]], "xin")
        for c in range(8):
            b_ = ps()
            for nb in range(2):
                k.tr(pv_(b_, S0, slice(128 * nb, 128 * nb + 128)), xt[:, nb, 128 * c:128 * c + 128], ident_f)
            k.cp("act" if c % 2 else "dve", memT[:, c, :], pv_(b_, S0, slice(0, NMEM)))
        for l in range(L):
            prenorm(memT, unT, 4, l, NMEM, rstd[:, 0:NMEM])
            w = load_unit("mem_%d" % l)
            for hp in range(2):
                b_ = fm_chunk(w, 128 * hp, rhs=unT, ncols=NMEM)
                k.cp("act", mk2T[l][:, hp, :], pv_(b_, S0, slice(0, NMEM)))
            for nb in range(2):
                b_ = ps()
                for kc in range(8):
                    k.mm(pv_(b_, S0, slice(0, 256)), unT[:, kc, 128 * nb:128 * nb + 128], w[:, kc, 256:512],
                         start=(kc == 0), stop=(kc == 7))
                src3 = View(psb[b_][:, 0:256].rearrange("p (h d) -> p h d", d=64), [("ps", b_)])
                k.cp("dve", mvdup[l][:, nb, :, 0:64], src3)
                k.cp("act", mvdup[l][:, nb, :, 64:128], src3)

    def load_block(s, blk):
        P.tag = 'load'
        for tt in range(4):
            r0 = blk * TB + 128 * tt
            k.dma("sp", xt[:, tt, :], x_d[s, r0:r0 + 128, :], "xin")
        sync_group([xt.all()], "xin")
        for c in range(8):
            b_ = ps()
            for tt in range(4):
                k.tr(pv_(b_, S0, slice(128 * tt, 128 * tt + 128)), xt[:, tt, 128 * c:128 * c + 128], ident_f)
            k.cp("act" if c % 2 else "dve", hT[:, c, :], pv_(b_))

    outkeys = []

    def store_block(s, blk):
        P.tag = 'store'
        for tt in range(4):
            b0, b1 = ps(), ps()
            for c in range(8):
                bb = b0 if c < 4 else b1
                k.tr(pv_(bb, S0, slice(128 * (c % 4), 128 * (c % 4) + 128)), hT[:, c, 128 * tt:128 * tt + 128], ident_f)
            k.cp("dve", xt[:, tt, 0:512], pv_(b0))
            k.cp("act", xt[:, tt, 512:1024], pv_(b1))
            r0 = blk * TB + 128 * tt
            key = "out%d" % len(outkeys)
            outkeys.append(key)
            k.dma("sp", out_d[s, r0:r0 + 128, :], xt[:, tt, :], "xout%d" % tt, writes=[key])

    def mix(s, blk, l):
        first = (blk == 0)
        W = 16 + TB
        lo_, up_ = slice(0, 64), slice(64, 128)
        P.tag = 'mix_norm'
        prenorm(hT, uT, 0, l, TB, rstd.all())
        P.tag = 'tok'
        wT = load_unit("inT_%d" % l, 8, 328)
        for tt in range(4):
            g = blk * 4 + tt
            b_ = ps()
            for kc in range(8):
                k.mm(pv_(b_, S0, slice(0, 328)), uT[:, kc, 128 * tt:128 * tt + 128], wT[:, kc, 0:328],
                     start=(kc == 0), stop=(kc == 7))
            k.cp("act", itok[:, tt, :], pv_(b_, S0, slice(0, 256)))
            for par in range(2):
                src3 = View(psb[b_][:, 0:256].rearrange("p (hp q d) -> p hp q d", q=2, d=64)[:, :, par, :],
                            [("ps", b_)])
                k.cp("dve", ipad[:, tt, slice(par, 4, 2), 64 * par:64 * par + 64], src3)
            k.cp("dve", vdup[l][:, g, 0:64], pv_(b_, S0, slice(256, 320)))
            k.cp("act", vdup[l][:, g, 64:128], pv_(b_, S0, slice(256, 320)))
            k.act(absw[:, tt, :], pv_(b_, S0, slice(320, 328)), AF.Abs, scale=IDX_C)
            k.act(sgn[:, tt, :], pv_(b_, S0, slice(320, 328)), AF.Sign)
        P.tag = 'gz'
        w2 = load_unit("in2_%d" % l)
        for hp in range(2):
            b_ = fm_chunk(w2, 128 * hp)
            k.act(gs[:, hp, :], pv_(b_), AF.Silu)
        for ci in range(2):
            if first:
                k.memset("pool", zbuf[:, ci, 0:16], 0.0)
            else:
                k.cp("pool", zbuf[:, ci, 0:16], halo[:, l, ci, :])
            b_ = fm_chunk(w2, 256 + 128 * ci)
            k.cp("act", zbuf[:, ci, 16:W], pv_(b_))
            k.cp("pool", halo[:, l, ci, :], zbuf[:, ci, TB:W])
        P.tag = 'hgrn'
        w0 = load_unit("in0_%d" % l)
        w1 = load_unit("in1_%d" % l)
        for h in range(4):
            if "a" not in en:
                break
            hh = h % 2
            bq = fm_chunk(w0, 128 * h)
            bf = fm_chunk(w1, 128 * h)
            lb, oml, noml = lbt[:, l, 0, h:h + 1], lbt[:, l, 1, h:h + 1], lbt[:, l, 2, h:h + 1]
            k.act(hA.all(), pv_(bf), AF.Sigmoid)
            k.act(hC.all(), hA.all(), AF.Identity, bias=oml, scale=noml)
            k.ts("dve", hA.all(), hA.all(), oml, lb, ALU.mult, ALU.add)
            k.act(hA.all(), hA.all(), AF.Ln)
            k.scan(hB.all(), c_("scanmask", 512), hA.all(), 0.0, ALU.mult, ALU.add)
            for c in range(8):
                cs = slice(64 * c, 64 * c + 64)
                k.ts("dve", hA2[:, cs], hB[:, cs], hB[:, 64 * c + 63:64 * c + 64], None, ALU.subtract)
            k.act(hE2.all(), hA2.all(), AF.Exp, scale=-1.0)
            k.tt("dve", hF.all(), hC.all(), hE2.all(), ALU.mult)
            k.act(hE.all(), hB.all(), AF.Exp)
            k.cp("dve", dcol.all(), View(hE.h[:, 63:512:64], hE.all().keys))
            bt = ps()
            for p_ in range(4):
                k.tr(pv_(bt, S0, slice(128 * p_, 128 * p_ + 128)), hF[:, 128 * p_:128 * p_ + 128], ident_f)
            src3 = psb[bt][:, :].rearrange("p (a q) -> p a q", q=128)
            k.cp("dve", ktok[lo_, :, 0, :], View(src3[0:64], [("ps", bt)]))
            k.cp("act", ktok[up_, :, 1, :], View(src3[64:128], [("ps", bt)]))
            bu = ps()
            for c in range(8):
                k.mm(pv_(bu, S0, slice(64 * c, 64 * c + 64)), ktok[:, c // 2, c % 2, :],
                     itok[:, c // 2, 64 * h:64 * h + 64])
            S = Sst[:, l, h, :]
            if first:
                k.memset("dve", S, 0.0)
            for c in range(8):
                k.cp("dve", Spad[hh][:, c, 64 * hh:64 * hh + 64], S)
                k.stt(S, S, dcol[:, c:c + 1], pv_(bu, S0, slice(64 * c, 64 * c + 64)), ALU.mult, ALU.add)
            k.act(hQ.all(), pv_(bq), AF.Silu)
            k.tt("pool", qS[hh].all(), hQ.all(), hE.all(), ALU.mult)
            for c in range(8):
                cs = slice(64 * c, 64 * c + 64)
                k.ts("dve", hA[:, cs], hB[:, cs], hB[:, 64 * c + 31:64 * c + 32], None, ALU.subtract)
            k.act(hE2.all(), hA.all(), AF.Exp)
            k.tt("dve", Qp.all(), hQ.all(), hE2.all(), ALU.mult)
            k.act(hE.all(), hA.all(), AF.Exp, scale=-1.0)
            k.tt("pool", Kp.all(), hC.all(), hE.all(), ALU.mult)
            ba = ps()
            for p_ in range(4):
                rg = slice(128 * p_, 128 * p_ + 128)
                k.mm(pv_(ba, S0, rg), Kp[:, rg], Qp[:, rg])
            k.tt("dve", attm[hh].all(), pv_(ba), c_("trimask", 512), ALU.mult)
            if hh == 1:
                hp = h // 2
                for p_ in range(4):
                    rg = slice(128 * p_, 128 * p_ + 128)
                    k.mm(pv_(6, S0, rg), ipad[:, p_, 2 * hp, :], attm[0][:, rg], start=True, stop=False)
                    k.mm(pv_(6, S0, rg), ipad[:, p_, 2 * hp + 1, :], attm[1][:, rg], start=False, stop=False)
                    for c in (2 * p_, 2 * p_ + 1):
                        for q in range(2):
                            cs = slice(64 * c, 64 * c + 64)
                            k.mm(pv_(6, S0, cs), Spad[q][:, c, :], qS[q][:, cs], start=False,
                                 stop=(c == 2 * p_ + 1 and q == 1))
                sq = sqr[0]
                k.act(sq.all(), pv_(6), AF.Square)
                bn = ps()
                k.mm(pv_(bn), bdones_b.all(), sq.all())
                k.act(tmpn.all(), pv_(bn), AF.Sqrt, bias=eps_c, scale=1.0 / 64)
                k.recip(oT.all(), tmpn.all())
                k.stt(oT.all(), pv_(6), pv_hgn(l, hp), oT.all(), ALU.mult, ALU.mult)
                k.tt("pool", yT["a"][:, hp, :], oT.all(), gs[:, hp, :], ALU.mult)
        P.tag = 'pool'
        if "b" in en:
            for ci in range(2):
                if ci == 0:
                    k.tt("pool", sfin[lo_, 1:W], zbuf[lo_, ci, 1:W], zbuf[lo_, ci, 0:W - 1], ALU.add)
                    k.tt("pool", s2b[up_, 1:W], zbuf[up_, ci, 1:W], zbuf[up_, ci, 0:W - 1], ALU.add)
                    k.tt("pool", sfin[up_, 3:W], s2b[up_, 3:W], s2b[up_, 1:W - 2], ALU.add)
                else:
                    k.tt("pool", s2b[:, 1:W], zbuf[:, ci, 1:W], zbuf[:, ci, 0:W - 1], ALU.add)
                    k.tt("pool", s4b[:, 3:W], s2b[:, 3:W], s2b[:, 1:W - 2], ALU.add)
                    k.tt("pool", sfin[lo_, 7:W], s4b[lo_, 7:W], s4b[lo_, 3:W - 4], ALU.add)
                    k.tt("pool", s8b[up_, 7:W], s4b[up_, 7:W], s4b[up_, 3:W - 4], ALU.add)
                    k.tt("pool", sfin[up_, 15:W], s8b[up_, 15:W], s8b[up_, 7:W - 8], ALU.add)
                o_ = C_OFF["invw"] + ci
                k.stt(pooled_b[:, ci, :], sfin[:, 16:W], cst[:, o_:o_ + 1], zbuf[:, ci, 16:W], ALU.mult, ALU.subtract)
                if first:
                    o2 = C_OFF["invcnt"] + 16 * ci
                    k.tt("dve", sfin[:, 0:16], sfin[:, 16:32], cst[:, o2:o2 + 16], ALU.mult)
                    k.tt("dve", pooled_b[:, ci, 0:16], sfin[:, 0:16], zbuf[:, ci, 16:32], ALU.subtract)
                b_ = ps()
                k.mm(pv_(b_), poolw_b[:, l, ci, :], pooled_b[:, ci, :])
                k.act(yT["b"][:, ci, :], pv_(b_), AF.Identity, scale=pv_psc(l, ci))
        P.tag = 'dsa_in'
        w3 = load_unit("in3_%d" % l)
        for hp in range(2):
            b_ = fm_chunk(w3, 128 * hp)
            k.act(sqpad[lo_, 2 * hp, :], pv_(b_, lo_), AF.Copy, scale=0.125)
            k.act(sqpad[up_, 2 * hp + 1, :], pv_(b_, up_), AF.Copy, scale=0.125)
        cb = slice(blk * TB, blk * TB + TB)
        b_ = fm_chunk(w3, 256)
        k.cp("dve", skdup[l][:, cb], pv_(b_))
        b_ = fm_chunk(w3, 384)
        k.cp("act", ikdup[l][:, cb], pv_(b_))
        w4 = load_unit("in4_%d" % l)
        for hp in range(4):
            b_ = fm_chunk(w4, 128 * hp)
            k.cp("dve", iqpad[lo_, 2 * hp, :], pv_(b_, lo_))
            k.cp("act", iqpad[up_, 2 * hp + 1, :], pv_(b_, up_))
        w5 = load_unit("in5_%d" % l, 8, 256)
        for hp in range(2):
            b_ = fm_chunk(w5, 128 * hp)
            k.act(mqpad[lo_, 2 * hp, :], pv_(b_, lo_), AF.Copy, scale=0.125)
            k.act(mqpad[up_, 2 * hp + 1, :], pv_(b_, up_), AF.Copy, scale=0.125)
        P.tag = 'dsa'
        def mem_attn():
            P.tag = 'mem'
            if "m" not in en:
                return
            for h in range(4):
                hh, hp = h % 2, h // 2
                for nb in range(2):
                    b_ = ps()
                    k.mm(pv_(b_), mk2T[l][:, hp, 128 * nb:128 * nb + 128], mqpad[:, h, :])
                    p_t = pT[nb]
                    k.act(p_t.all(), pv_(b_), AF.Exp)
                    k.mm(pv_(6), mvdup[l][:, nb, h, :], p_t.all(), start=(nb == 0), stop=(nb == 1))
                for nb in range(2):
                    k.mm(pv_(7), ones_b.all(), pT[nb].all(), start=(nb == 0), stop=(nb == 1))
                rows = lo_ if hh == 0 else up_
                k.recip(rec[rows, :], pv_(7, rows))
                k.tt("dve", yT["m"][rows, hp, :], pv_(6, rows), rec[rows, :], ALU.mult)
            P.tag = 'dsa'

        if "c" in en:
            dsa_score(l, blk, 0)
            dsa_bisect(l, blk, 0)
            mem_attn()
            for tt in range(1, 4):
                dsa_score(l, blk, tt)
                dsa_bisect(l, blk, tt)
                dsa_attend(l, blk, tt - 1)
            dsa_attend(l, blk, 3)
        else:
            mem_attn()
        P.tag = 'mem'
        P.tag = 'gates'
        for half in range(2):
            src = scr["br%d_%d" % (half, l)]
            P.add("sp", (lambda src=src: (lambda e: e.dma_start(out=wbr.all().ap, in_=src)))(),
                  reads=["br%d_%d" % (half, l)], writes=[wbr.all()], dma="wbr")
            for bi, b_name in enumerate("abcm"):
                wg = load_unit("g%d_%d_%d" % (bi, half, l))
                for fq in range(4):
                    fc = 4 * half + fq
                    bg = fm_chunk(wg, 128 * fq)
                    sg_ = sig[fq % 2]
                    k.act(sg_.all(), pv_(bg), AF.Sigmoid)
                    bb = ps()
                    for kc in range(2):
                        k.mm(pv_(bb), wbr[:, 2 * bi + kc, 128 * fq:128 * fq + 128], yT[b_name][:, kc, :],
                             start=(kc == 0), stop=(kc == 1))
                    if bi == 0:
                        k.tt("dve", bigf[:, fc, :], pv_(bb), sg_.all(), ALU.mult)
                    else:
                        t_ = tb[fq % 2]
                        k.tt("dve", t_.all(), pv_(bb), sg_.all(), ALU.mult)
                        if bi < 3:
                            k.tt("pool", bigf[:, fc, :], bigf[:, fc, :], t_.all(), ALU.add)
                        else:
                            k.tt("pool", merged_b[:, fc, :], bigf[:, fc, :], t_.all(), ALU.add)
        P.tag = 'wout'
        for half in range(2):
            wo = load_unit("o%d_%d" % (half, l))
            for fq in range(4):
                fc = 4 * half + fq
                b_ = fm_chunk(wo, 128 * fq, rhs=merged_b)
                post_evac(b_, fc, l, 1)
        post_finish()

    def dsa_score(l, blk, tt):
        g = blk * 4 + tt
        par = g % 2
        N = (g + 1) * 128
        qs = slice(128 * tt, 128 * tt + 128)
        sc = score[par]
        if g < 2:
            return
        for h in range(8):
            k.ts("dve", Dh[:, h, :], ident_b.all(), sgn[:, tt, h:h + 1], None, ALU.mult)
        npiece = (N + 511) // 512
        items = [(j, h) for j in range(npiece) for h in range(8)]
        bl_of = {}

        def geo(j):
            w_ = min(512, N - 512 * j)
            return slice(512 * j, 512 * j + w_), slice(0, w_)

        def logits(i):
            j, h = items[i]
            ks_, ws_ = geo(j)
            bl_of[i] = ps()
            k.mm(pv_(bl_of[i], S0, ws_), iqpad[:, h, qs], ikdup[l][:, ks_])

        LA = 3
        for i in range(min(LA, len(items))):
            logits(i)
        for i, (j, h) in enumerate(items):
            ks_, ws_ = geo(j)
            bsc = 6 + (j % 2)
            r_ = rh[i % 3]
            k.act(r_[:, ws_], pv_(bl_of[i], S0, ws_), AF.Relu, scale=absw[:, tt, h:h + 1])
            if i + LA < len(items):
                logits(i + LA)
            k.mm(pv_(bsc, S0, ws_), Dh[:, h, :], r_[:, ws_], start=(h == 0), stop=(h == 7))
            if h == 7:
                k.cp("act", sc[:, ks_], pv_(bsc, S0, ws_))

    def dsa_bisect(l, blk, tt):
        g = blk * 4 + tt
        par = g % 2
        N = (g + 1) * 128
        sc, mb = score[par], mbias[par]
        if g < 2:
            if N > 128:
                k.memset("pool", mb[:, 0:N - 128], 0.0)
            k.cp("dve", mb[:, N - 128:N], c_("mbdiag", 128))
            return
        A_, lo_c, mid_c, cnt_c, inc_c = [cols[:, par, i:i + 1] for i in range(5)]
        hw = cols[:, par, 8:8 + NBIS]
        k.reduce(A_, sc[:, 0:N], ALU.max, absval=True)
        k.tt("pool", sc[:, N - 128:N], sc[:, N - 128:N], c_("negadm", 128), ALU.add)
        k.ts("dve", lo_c, A_, -1.0, None, ALU.mult)
        k.ts("dve", hw, c_("pow2", NBIS), A_, None, ALU.mult)
        for it in range(NBIS):
            s_i = cols[:, par, 8 + it:9 + it]
            k.ts("dve", mid_c, lo_c, s_i, None, ALU.add)
            k.ts("dve", mb[:, 0:N], sc[:, 0:N], mid_c, None, ALU.is_ge, ALU.add, accum=cnt_c)
            k.ts("dve", inc_c, cnt_c, 256.0, s_i, ALU.is_ge, ALU.mult)
            k.tt("dve", lo_c, lo_c, inc_c, ALU.add)
        k.ts("dve", mb[:, 0:N], sc[:, 0:N], lo_c, -30000.0, ALU.is_lt, ALU.mult)

    def dsa_attend(l, blk, tt):
        g = blk * 4 + tt
        par = g % 2
        qs = slice(128 * tt, 128 * tt + 128)
        lo_, up_ = slice(0, 64), slice(64, 128)
        mb = mbias[par]
        qk_bank = {}

        def qk(kb):
            ksl = slice(128 * kb, 128 * kb + 128)
            b_ = ps()
            qk_bank[kb] = b_
            for h in range(4):
                hs = slice(128 * h, 128 * h + 128)
                k.mm(pv_(b_, S0, hs), mb[:, ksl], ident_b.all(), start=True, stop=False)
                k.mm(pv_(b_, S0, hs), skdup[l][:, ksl], sqpad[:, h, qs], start=False, stop=True)

        qk(0)
        for kb in range(g + 1):
            p_t = pT[kb % 2]
            k.act(p_t.all(), pv_(qk_bank[kb]), AF.Exp)
            if kb + 1 <= g:
                qk(kb + 1)
            k.mm(pv_(6), vdup[l][:, kb, :], p_t.all(), start=(kb == 0), stop=(kb == g))
            k.mm(pv_(7), ones_b.all(), p_t.all(), start=(kb == 0), stop=(kb == g))
        k.recip(rec.all(), pv_(7))
        for h in range(4):
            rows = lo_ if h % 2 == 0 else up_
            hs = slice(128 * h, 128 * h + 128)
            k.tt("dve", yT["c"][rows, h // 2, qs], pv_(6, rows, hs), rec[rows, hs], ALU.mult)

    def ffn(l):
        import os
        kf = int(os.environ.get("KF", "9"))
        P.tag = 'ffn_norm'
        prenorm(hT, uT, 2, l, TB, rstd.all())
        P.tag = 'ffn_gu'
        if kf < 2:
            return
        for i in range(11):
            w = load_unit("gu%d_%d" % (i, l))
            for q in range(2):
                j = 2 * i + q
                bg = fm_chunk(w, 128 * q)
                bu = fm_chunk(w, 256 + 128 * q)
                s_ = sgt[j % 2]
                k.act(s_.all(), pv_(bg), AF.Silu)
                k.tt("dve", actT[:, j, :], pv_(bu), s_.all(), ALU.mult)
        if kf < 3:
            return
        P.tag = 'ffn_down'
        for half in range(2):
            wd = [load_unit("d%d_%d_%d" % (jg, half, l), min(8, 22 - 8 * jg), 512) for jg in range(3)]
            for fq in range(4):
                fc = 4 * half + fq
                b_ = ps()
                for j in range(22):
                    k.mm(pv_(b_), wd[j // 8][:, j % 8, 128 * fq:128 * fq + 128], actT[:, j, :],
                         start=(j == 0), stop=(j == 21))
                post_evac(b_, fc, l, 3)
        if kf < 4:
            return
        post_finish()

    for s in range(nseq):
        if "m" in en:
            mem_setup(s)
        for blk in range(NBLK):
            load_block(s, blk)
            import os
            dbg = os.environ.get("KDBG", "mf")
            for l in range(L):
                if "m" in dbg:
                    mix(s, blk, l)
                if "f" in dbg:
                    ffn(l)
            store_block(s, blk)
    P.add("sp", lambda e: e.nop(), reads=outkeys)
    P.emit()
    return nc, P


_CACHE = {}


def run(inputs, nseq, T, L, ncores, en=("a", "b", "c", "m"), trace=False):
    key = (nseq, T, L, tuple(en))
    if key not in _CACHE:
        _CACHE[key] = build(nseq, T, L, en)[0]
    nc = _CACHE[key]
    f = lambda a: np.ascontiguousarray(np.asarray(a), dtype=np.float32)
    x = f(inputs["x"])
    mem = f(inputs["mem"])
    shared = {
        "consts": CONSTS,
        "pvec": _mk_pvec(inputs, L),
        "poolw": _mk_poolw(f(inputs["pool_w"]), L),
        "w_in": f(inputs["w_in"])[:L],
        "w_gate": f(inputs["w_gate"])[:L],
        "w_branch": f(inputs["w_branch"])[:L],
        "w_out": f(inputs["w_out"])[:L],
        "w_mem_kv": f(inputs["w_mem_kv"])[:L],
        "w_ffn_gate_up": f(inputs["w_ffn_gate_up"])[:L],
        "w_ffn_down": f(inputs["w_ffn_down"])[:L],
    }
    in_maps = []
    for c in range(ncores):
        m = dict(shared)
        m["x"] = np.ascontiguousarray(x[c * nseq:(c + 1) * nseq, :T])
        m["mem"] = np.ascontiguousarray(mem[c * nseq:(c + 1) * nseq])
        in_maps.append(m)
    res = run_bass_kernel_spmd(nc, in_maps, core_ids=list(range(ncores)), trace=trace)
    out = np.concatenate([np.asarray(r["out"]) for r in res.results], axis=0)
    return out.astype(np.float32), res


def kernel(**inputs):
    out, _ = run(inputs, 2, 2048, 2, 8)
    return out
```

```python
import numpy as np
import concourse.bass as bass
import concourse.mybir as mybir
from concourse.bass_utils import run_bass_kernel_spmd

F32 = mybir.dt.float32
BF16 = mybir.dt.bfloat16
AF = mybir.ActivationFunctionType
ALU = mybir.AluOpType
AX = mybir.AxisListType

D = 1024
TB = 512
DFF = 2816
NIN = 3016
NMEM = 256
EPS = 1e-6
PAGE = 64
NBIS = 16
DVE_INORDER = False


class View:
    __slots__ = ("ap", "keys")

    def __init__(self, ap, keys):
        self.ap = ap
        self.keys = keys


class Buf:
    def __init__(self, nc, name, shape, dtype, off):
        self.shape = list(shape)
        self.esz = 4 if dtype == F32 else 2
        self.off = off
        self.h = nc.alloc_sbuf_tensor_at(name, self.shape, dtype, offset=off)
        self.strides = []
        s = 1
        for d in reversed(self.shape[1:]):
            self.strides.insert(0, s)
            s *= d
        self.nelem = s
        self.nbytes = s * self.esz

    def __getitem__(self, idx):
        if not isinstance(idx, tuple):
            idx = (idx,)
        lo = 0
        hi = 0
        for k, st in enumerate(self.strides):
            dim = self.shape[k + 1]
            if k + 1 < len(idx):
                i = idx[k + 1]
                if isinstance(i, slice):
                    a = 0 if i.start is None else i.start
                    b = dim if i.stop is None else i.stop
                else:
                    a, b = i, i + 1
            else:
                a, b = 0, dim
            lo += a * st
            hi += (b - 1) * st
        hi += 1
        b0 = (self.off + lo * self.esz) // PAGE
        b1 = (self.off + hi * self.esz - 1) // PAGE
        return View(self.h[idx], [("sb", p) for p in range(b0, b1 + 1)])

    def all(self):
        return self[(slice(None),) * len(self.shape)]


class Prog:
    ENG = ("sp", "pe", "act", "dve", "pool")

    def __init__(self, nc):
        self.nc = nc
        self.ops = {e: [] for e in self.ENG}
        self.cnt = {}
        self.sem = {}
        self.res = {}
        self.known = {e: {} for e in self.ENG}
        self.epoch = {}
        self.nops = 0
        self.tag = ''
        self.tags = {e: [] for e in self.ENG}

    def _sem(self, key):
        if key not in self.sem:
            self.sem[key] = self.nc.alloc_semaphore("s_" + key)
            self.cnt[key] = 0

    def add(self, eng, fn, reads=(), writes=(), dma=None):
        deps = set()
        rk = []
        wk = []
        for v in reads:
            for key in (v.keys if isinstance(v, View) else [v]):
                if isinstance(key, tuple) and key[0] == "ps":
                    wk.append(key)
                else:
                    rk.append(key)
        for v in writes:
            wk.extend(v.keys if isinstance(v, View) else [v])
        for k in rk:
            r = self.res.get(k)
            if r is not None and r[0] is not None:
                deps.add(r[0])
        for k in wk:
            r = self.res.get(k)
            if r is not None:
                if r[0] is not None:
                    deps.add(r[0])
                deps.update(r[1])
        if dma:
            semkey = dma
        else:
            ep = self.epoch.get(eng, 0)
            semkey = "%s_%d" % (eng, ep)
            if self.cnt.get(semkey, 0) >= 4000:
                self.epoch[eng] = ep + 1
                semkey = "%s_%d" % (eng, ep + 1)
        self._sem(semkey)
        inc = 16 if dma else 1
        self.cnt[semkey] += inc
        tok = (semkey, self.cnt[semkey])
        waits = {}
        kn = self.known[eng]
        for (sk, v) in deps:
            if eng == "pe" and sk.startswith("pe_"):
                continue
            if eng == "dve" and sk.startswith("dve_") and DVE_INORDER:
                continue
            if kn.get(sk, 0) >= v:
                continue
            if waits.get(sk, 0) < v:
                waits[sk] = v
        for sk, v in waits.items():
            kn[sk] = v
        self.ops[eng].append((list(waits.items()), fn, semkey, inc))
        self.tags[eng].append(self.tag)
        self.nops += 1
        for k in rk:
            r = self.res.get(k)
            if r is None:
                self.res[k] = [None, [tok]]
            else:
                r[1].append(tok)
        for k in wk:
            self.res[k] = [tok, []]
        return tok

    def emit(self):
        nc = self.nc

        def mk(eng):
            def body(e):
                for waits, fn, semkey, inc in self.ops[eng]:
                    for sk, v in waits:
                        e.wait_ge(self.sem[sk], v)
                    fn(e).then_inc(self.sem[semkey], inc)
            return body

        with nc.Block() as block:
            block.sync(mk("sp"))
            block.tensor(mk("pe"))
            block.scalar(mk("act"))
            block.vector(mk("dve"))
            block.gpsimd(mk("pool"))


def _a(v):
    return v.ap if isinstance(v, View) else v


def _vs(*xs):
    return [x for x in xs if isinstance(x, View)]


class K:
    def __init__(self, P):
        self.P = P

    def mm(self, out, lhsT, rhs, start=True, stop=True):
        self.P.add("pe", lambda e: e.matmul(out.ap, lhsT.ap, rhs.ap, start=start, stop=stop),
                   reads=[lhsT, rhs], writes=[out])

    def tr(self, out, in_, ident):
        self.P.add("pe", lambda e: e.transpose(out.ap, in_.ap, ident.ap), reads=[in_, ident], writes=[out])

    def act(self, out, in_, func, bias=None, scale=None, accum=None):
        kw = {}
        if bias is not None:
            kw["bias"] = _a(bias)
        if scale is not None:
            kw["scale"] = _a(scale)
        if accum is not None:
            kw["accum_out"] = accum.ap
        self.P.add("act", lambda e: e.activation(out.ap, in_.ap, func, **kw),
                   reads=_vs(in_, bias, scale), writes=_vs(out, accum))

    def ts(self, eng, out, in0, s1, s2, op0, op1=None, accum=None):
        kw = {}
        if op1 is not None:
            kw["op1"] = op1
        if accum is not None:
            kw["accum_out"] = accum.ap
        self.P.add(eng, lambda e: e.tensor_scalar(out.ap, in0.ap, _a(s1), _a(s2), op0, **kw),
                   reads=_vs(in0, s1, s2), writes=_vs(out, accum))

    def tt(self, eng, out, in0, in1, op):
        self.P.add(eng, lambda e: e.tensor_tensor(out.ap, in0.ap, in1.ap, op), reads=[in0, in1], writes=[out])

    def stt(self, out, in0, s, in1, op0, op1):
        self.P.add("dve", lambda e: e.scalar_tensor_tensor(out.ap, in0.ap, _a(s), in1.ap, op0, op1),
                   reads=_vs(in0, s, in1), writes=[out])

    def cp(self, eng, out, in_):
        if eng == "act":
            self.P.add("act", lambda e: e.copy(out.ap, in_.ap), reads=[in_], writes=[out])
        else:
            self.P.add(eng, lambda e: e.tensor_copy(out.ap, in_.ap), reads=[in_], writes=[out])

    def recip(self, out, in_):
        self.P.add("dve", lambda e: e.reciprocal(out.ap, in_.ap), reads=[in_], writes=[out])

    def memset(self, eng, out, val):
        self.P.add(eng, lambda e: e.memset(out.ap, val), writes=[out])

    def reduce(self, out, in_, op, axis=AX.X, absval=None):
        self.P.add("dve", lambda e: e.tensor_reduce(out.ap, in_.ap, axis, op, apply_absolute_value=absval),
                   reads=[in_], writes=[out])

    def scan(self, out, d0, d1, init, op0, op1):
        self.P.add("dve", lambda e: e.tensor_tensor_scan(out.ap, d0.ap, d1.ap, _a(init), op0, op1),
                   reads=_vs(d0, d1, init), writes=[out])

    def dma(self, q, out, in_, sem, reads=(), writes=()):
        self.P.add(q, lambda e: e.dma_start(out=_a(out), in_=_a(in_)),
                   reads=list(reads) + _vs(in_), writes=list(writes) + _vs(out), dma=sem)


C_OFF = {}


def _mk_consts():
    cols = []

    def put(name, arr):
        C_OFF[name] = sum(a.shape[1] for a in cols)
        cols.append(arr.astype(np.float32))

    p = np.arange(128)
    put("ident", np.eye(128))
    put("ones", np.ones((128, 128)))
    bd = np.zeros((128, 128))
    bd[:64, :64] = 1
    bd[64:, 64:] = 1
    put("bdones", bd)
    t = np.arange(512)
    put("scanmask", np.tile((t % 64 != 0).astype(np.float32)[None, :], (128, 1)))
    s_ = p[:, None]
    t_ = np.arange(128)[None, :]
    tri = ((s_ // 64 == t_ // 64) & (t_ >= s_)).astype(np.float32)
    put("trimask", np.tile(tri, (1, 4)))
    inad = (p[:, None] < 64) & (np.arange(128)[None, :] >= 64)
    put("negadm", np.where(inad, -1e30, 0.0))
    put("mbdiag", np.where(inad, -30000.0, 0.0))
    ic = np.zeros((128, 2, 16))
    ws = [[2, 4], [8, 16]]
    for ci in range(2):
        for half in range(2):
            w = ws[ci][half]
            ic[half * 64:(half + 1) * 64, ci, :] = 1.0 / np.minimum(np.arange(16) + 1, w)[None, :]
    put("invcnt", ic.reshape(128, 32))
    iw = np.zeros((128, 2))
    for ci in range(2):
        for half in range(2):
            iw[half * 64:(half + 1) * 64, ci] = 1.0 / ws[ci][half]
    put("invw", iw)
    put("eps", np.full((128, 1), EPS))
    put("c256", np.full((128, 1), 256.0))
    put("pow2", np.tile((2.0 ** -np.arange(24))[None, :], (128, 1)))
    return np.ascontiguousarray(np.concatenate(cols, axis=1), dtype=np.float32)


CONSTS = _mk_consts()
NCONST = CONSTS.shape[1]
PV_PER_L = 44


def _mk_pvec(inp, L):
    cols = []
    for l in range(L):
        for nm in ("norm_mix_pre", "norm_mix_post", "norm_ffn_pre", "norm_ffn_post", "norm_mem"):
            cols.append(np.asarray(inp[nm][l]).reshape(8, 128).T)
        cols.append(np.asarray(inp["hgrn_norm"][l]).reshape(2, 128).T)
        cols.append(np.asarray(inp["pool_scale"][l]).reshape(2, 128).T)
    for l in range(L):
        cols.append(np.asarray(inp["lb_logits"][l]).reshape(4, 128).T)
    return np.ascontiguousarray(np.concatenate(cols, axis=1), dtype=np.float32)


def _mk_poolw(pw, L):
    out = np.zeros((L, 128, 2, 128), np.float32)
    for l in range(L):
        for g in range(4):
            ci, half = g // 2, g % 2
            out[l, half * 64:(half + 1) * 64, ci, half * 64:(half + 1) * 64] = pw[l, g]
    return out


def build(nseq, T, L=2, en=("a", "b", "c", "m")):
    nc = bass.Bass("TRN2", target_bir_lowering=False)
    NBLK = T // TB
    NT = T // 128
    P = Prog(nc)
    k = K(P)

    def din(name, shape):
        return nc.dram_tensor(name, list(shape), F32, kind="ExternalInput").ap()

    x_d = din("x", [nseq, T, D])
    mem_d = din("mem", [nseq, NMEM, D])
    consts_d = din("consts", [128, NCONST])
    NPV = PV_PER_L * L + 4 * L
    pvec_d = din("pvec", [128, NPV])
    poolw_d = din("poolw", [L, 128, 2, 128])
    w_in_d = din("w_in", [L, D, NIN])
    w_gate_d = din("w_gate", [L, 4, D, D])
    w_branch_d = din("w_branch", [L, 4, 256, D])
    w_out_d = din("w_out", [L, D, D])
    w_mem_d = din("w_mem_kv", [L, D, 512])
    w_gu_d = din("w_ffn_gate_up", [L, D, 2 * DFF])
    w_dn_d = din("w_ffn_down", [L, DFF, D])
    out_d = nc.dram_tensor("out", [nseq, T, D], F32, kind="ExternalOutput").ap()

    off = [((nc.sbuf_base + 63) // 64) * 64]
    sb_top = nc.sbuf_top

    def alloc(name, shape, dtype, at=None):
        esz = 4 if dtype == F32 else 2
        n = 1
        for d_ in shape[1:]:
            n *= d_
        nb = ((n * esz + 63) // 64) * 64
        if at is None:
            o = off[0]
            off[0] += nb
        else:
            o = at
        assert o + nb <= sb_top, (name, o, nb, sb_top)
        return Buf(nc, name, shape, dtype, o)

    cst = alloc("cst", [128, NCONST], F32)
    pv = alloc("pv", [128, NPV], F32)
    lbt = alloc("lbt", [128, L, 3, 4], F32)
    ident_b = alloc("ident_b", [128, 128], BF16)
    ones_b = alloc("ones_b", [128, 128], BF16)
    bdones_b = alloc("bdones_b", [128, 128], BF16)
    poolw_b = alloc("poolw_b", [128, L, 2, 128], BF16)
    hT = alloc("hT", [128, 8, TB], F32)
    uT = alloc("uT", [128, 8, TB], BF16)
    rstd = alloc("rstd", [128, TB], F32)
    bigf = alloc("bigf", [128, 8, TB], F32)
    xt = Buf(nc, "xt", [128, 4, D], F32, bigf.off)
    merged_b = alloc("merged_b", [128, 8, TB], BF16)
    ring = [alloc("ring%d" % i, [128, 8, 512], BF16) for i in range(4)]
    wbr = alloc("wbr", [128, 8, 512], BF16)
    yT = {b_: alloc("yT_" + b_, [128, 2, TB], BF16) for b_ in "abcm"}
    ikdup = [alloc("ikdup%d" % l, [128, T], BF16) for l in range(L)]
    skdup = [alloc("skdup%d" % l, [128, T], BF16) for l in range(L)]
    vdup = [alloc("vdup%d" % l, [128, NT, 128], BF16) for l in range(L)]
    Sst = alloc("Sst", [128, L, 4, 64], F32)
    halo = alloc("halo", [128, L, 2, 16], F32)
    mk2T = [alloc("mk2T%d" % l, [128, 2, NMEM], BF16) for l in range(L)]
    mvdup = [alloc("mvdup%d" % l, [128, 2, 4, 128], BF16) for l in range(L)]
    sqr = [alloc("sqr%d" % i, [128, TB], BF16) for i in range(2)]
    tmpn = alloc("tmpn", [128, TB], F32)
    zbuf = alloc("zbuf", [128, 2, 16 + TB], F32)
    sqpad = alloc("sqpad", [128, 4, TB], BF16)
    iqpad = alloc("iqpad", [128, 8, TB], BF16)
    mqpad = alloc("mqpad", [128, 4, TB], BF16)
    absw = alloc("absw", [128, 4, 8], F32)
    sgn = alloc("sgn", [128, 4, 8], F32)
    ipad = alloc("ipad", [128, 4, 4, 128], BF16)
    cols = alloc("cols", [128, 2, 32], F32)
    Spad = [alloc("Spad%d" % i, [128, 8, 128], BF16) for i in range(2)]
    ktok = alloc("ktok", [128, 4, 2, 128], BF16)
    arena0 = off[0]

    def phase():
        off[0] = arena0

    phase()
    itok = alloc("itok", [128, 4, 256], BF16)
    gs = alloc("gs", [128, 2, TB], BF16)
    hA = alloc("hA", [128, TB], F32)
    hB = alloc("hB", [128, TB], F32)
    hC = alloc("hC", [128, TB], F32)
    hQ = alloc("hQ", [128, TB], F32)
    hE = alloc("hE", [128, TB], F32)
    hF = alloc("hF", [128, TB], F32)
    hA2 = alloc("hA2", [128, TB], F32)
    hE2 = alloc("hE2", [128, TB], F32)
    Qp = alloc("Qp", [128, TB], BF16)
    Kp = alloc("Kp", [128, TB], BF16)
    qS = [alloc("qS%d" % i, [128, TB], BF16) for i in range(2)]
    attm = [alloc("attm%d" % i, [128, TB], BF16) for i in range(2)]
    dcol = alloc("dcol", [128, 8], F32)
    oT = alloc("oT", [128, TB], F32)
    end_h = off[0]
    phase()
    s2b = alloc("s2b", [128, 16 + TB], F32)
    s4b = alloc("s4b", [128, 16 + TB], F32)
    s8b = alloc("s8b", [128, 16 + TB], F32)
    sfin = alloc("sfin", [128, 16 + TB], F32)
    pooled_b = alloc("pooled_b", [128, 2, TB], BF16)
    end_p = off[0]
    phase()
    Dh = alloc("Dh", [128, 8, 128], BF16)
    rh = [alloc("rh%d" % i, [128, 512], BF16) for i in range(3)]
    score = [alloc("score%d" % i, [128, T], F32) for i in range(2)]
    mbias = [alloc("mbias%d" % i, [128, T], BF16) for i in range(2)]
    pT = [alloc("pT%d" % i, [128, 512], BF16) for i in range(2)]
    rec = alloc("rec", [128, 512], F32)
    end_d = off[0]
    phase()
    memT = alloc("memT", [128, 8, NMEM], F32)
    unT = alloc("unT", [128, 8, NMEM], BF16)
    end_m = off[0]
    phase()
    sig = [alloc("sig%d" % i, [128, TB], BF16) for i in range(2)]
    tb = [alloc("tb%d" % i, [128, TB], F32) for i in range(2)]
    end_g = off[0]
    phase()
    actT = alloc("actT", [128, 22, TB], BF16)
    sgt = [alloc("sgt%d" % i, [128, TB], BF16) for i in range(2)]
    end_f = off[0]
    print('SBUF use', arena0, max(end_h, end_p, end_d, end_m, end_g, end_f), sb_top)
    assert max(end_h, end_p, end_d, end_m, end_g, end_f) <= sb_top

    psb = [nc.alloc_psum_tensor("ps%d" % i, [128, 512], F32) for i in range(8)]
    rr = [0]

    def ps(bank=None):
        if bank is None:
            bank = rr[0] % 6
            rr[0] += 1
        return bank

    def pv_(bank, *idx):
        if not idx:
            idx = (slice(None), slice(None))
        return View(psb[bank][idx], [("ps", bank)])

    def c_(name, n, rows=slice(None)):
        o = C_OFF[name]
        return cst[rows, o:o + n]

    ident_f = c_("ident", 128)
    eps_c = c_("eps", 1)

    def pvc(l, grp, j):
        o = PV_PER_L * l + grp * 8 + j
        return pv[:, o:o + 1]

    def pv_hgn(l, j):
        o = PV_PER_L * l + 40 + j
        return pv[:, o:o + 1]

    def pv_psc(l, j):
        o = PV_PER_L * l + 42 + j
        return pv[:, o:o + 1]

    scr = {}

    def mk_scr(name, nk, ncols):
        scr[name] = nc.dram_tensor("scr_" + name, [128, nk, ncols], BF16, kind="Internal").ap()

    cast_i = [0]

    def cast(name, c0, src, sem):
        import os
        if os.environ.get("KNOCAST"):
            return
        n = src.shape[-1]
        cast_i[0] += 1
        dst = scr[name][:, 0:src.shape[1], c0:c0 + n]
        P.add("pool", lambda e: e.dma_start(out=dst, in_=src), reads=[], writes=["cast%d" % cast_i[0]], dma=sem)

    def finish_cast(names, sem):
        if sem not in P.cnt:
            return
        tok = (sem, P.cnt[sem])
        for nm in names:
            P.res[nm] = [tok, []]

    def rows_kc(ap2d):
        return ap2d.rearrange("(kc p) n -> p kc n", p=128)

    def sync_group(views, sem):
        tok = (sem, P.cnt[sem])
        for v in views:
            for key in v.keys:
                P.res[key] = [tok, []]

    P.tag = 'setup'
    k.dma("sp", cst.all(), consts_d, "init")
    k.dma("sp", pv.all(), pvec_d, "init")
    sync_group([cst.all(), pv.all()], "init")
    k.cp("dve", ident_b.all(), ident_f)
    k.cp("dve", ones_b.all(), c_("ones", 128))
    k.cp("dve", bdones_b.all(), c_("bdones", 128))
    for buf in (sqpad, iqpad, mqpad, ipad, ktok, Spad[0], Spad[1]):
        k.memset("pool", buf.all(), 0.0)
    lo_ = PV_PER_L * L
    for l in range(L):
        if l == 0:
            k.memset("dve", lbt[:, l, 0, :], 0.0)
        else:
            assert L == 2
            k.tt("dve", lbt[:, l, 1, :], pv[:, lo_ + 4:lo_ + 8], pv[:, lo_:lo_ + 4], ALU.subtract)
            k.act(lbt[:, l, 0, :], lbt[:, l, 1, :], AF.Sigmoid)
        k.ts("dve", lbt[:, l, 1, :], lbt[:, l, 0, :], -1.0, 1.0, ALU.mult, ALU.add)
        k.ts("dve", lbt[:, l, 2, :], lbt[:, l, 1, :], -1.0, None, ALU.mult)
    for l in range(L):
        k.dma("sp", tmpn[:, 0:256], poolw_d[l].rearrange("p a b -> p (a b)"), "init2")
        k.cp("dve", poolw_b[:, l, :, :], View(tmpn.h[:, 0:256].rearrange("p (a b) -> p a b", b=128), tmpn[:, 0:256].keys))

    stage = [Buf(nc, "stage0", [128, 8, 512], F32, bigf.off), Buf(nc, "stage1", [128, 8, 512], F32, hT.off),
             Buf(nc, "stage2", [128, 8, 512], F32, arena0), Buf(nc, "stage3", [128, 8, 512], F32, arena0 + 16384)]
    cast_n = [0]

    def cast_unit(nm, nk, ncols, pieces):
        mk_scr(nm, nk, ncols)
        i = cast_n[0]
        cast_n[0] += 1
        stg = stage[i % 4]
        for (k0, c0, s_ap) in pieces:
            nkk, n = s_ap.shape[1], s_ap.shape[2]
            k.dma("sp", stg[:, k0:k0 + nkk, c0:c0 + n], s_ap, "cin%d" % (i % 4))
        sync_group([stg.all()], "cin%d" % (i % 4))
        rb = ring[i % 4][:, 0:nk, 0:ncols]
        k.cp(("dve", "act", "dve", "act", "pool")[i % 5], rb, stg[:, 0:nk, 0:ncols])
        dst = scr[nm]
        P.add("sp", lambda e: e.dma_start(out=dst, in_=rb.ap), reads=[rb], writes=[nm], dma="cout%d" % (i % 4))

    for l in range(L):
        wi = rows_kc(w_in_d[l])
        cast_unit("inT_%d" % l, 8, 328, [(0, 0, wi[:, :, 1024:1280]), (0, 256, wi[:, :, 2112:2176]),
                                         (0, 320, wi[:, :, 2752:2760])])
        cast_unit("in2_%d" % l, 8, 512, [(0, 0, wi[:, :, 1280:1792])])
        cast_unit("in0_%d" % l, 8, 512, [(0, 0, wi[:, :, 0:512])])
        cast_unit("in1_%d" % l, 8, 512, [(0, 0, wi[:, :, 512:1024])])
        cast_unit("in3_%d" % l, 8, 512, [(0, 0, wi[:, :, 1792:2048]), (0, 256, wi[:, :, 2048:2112]),
                                         (0, 320, wi[:, :, 2048:2112]), (0, 384, wi[:, :, 2688:2752]),
                                         (0, 448, wi[:, :, 2688:2752])])
        cast_unit("in4_%d" % l, 8, 512, [(0, 0, wi[:, :, 2176:2688])])
        cast_unit("in5_%d" % l, 8, 256, [(0, 0, wi[:, :, 2760:3016])])
        cast_unit("mem_%d" % l, 8, 512, [(0, 0, rows_kc(w_mem_d[l]))])
        for half in range(2):
            cast_unit("br%d_%d" % (half, l), 8, 512,
                      [(2 * b_, 0, rows_kc(w_branch_d[l, b_])[:, :, 512 * half:512 * half + 512]) for b_ in range(4)])
        for half in range(2):
            for b_ in range(4):
                cast_unit("g%d_%d_%d" % (b_, half, l), 8, 512,
                          [(0, 0, rows_kc(w_gate_d[l, b_])[:, :, 512 * half:512 * half + 512])])
        for half in range(2):
            cast_unit("o%d_%d" % (half, l), 8, 512, [(0, 0, rows_kc(w_out_d[l])[:, :, 512 * half:512 * half + 512])])
        wgu = rows_kc(w_gu_d[l])
        for i in range(11):
            cast_unit("gu%d_%d" % (i, l), 8, 512, [(0, 0, wgu[:, :, 256 * i:256 * i + 256]),
                                                  (0, 256, wgu[:, :, DFF + 256 * i:DFF + 256 * i + 256])])
        wdn = w_dn_d[l].rearrange("(j p) n -> p j n", p=128)
        for half in range(2):
            for jg in range(3):
                nj = min(8, 22 - 8 * jg)
                cast_unit("d%d_%d_%d" % (jg, half, l), nj, 512,
                          [(0, 0, wdn[:, 8 * jg:8 * jg + nj, 512 * half:512 * half + 512])])

    rslot = [0]

    def load_unit(name, nk=8, ncols=512):
        s_ = rslot[0] % 4
        rslot[0] += 1
        dst = ring[s_][:, 0:nk, 0:ncols]
        src = scr[name][:, 0:nk, 0:ncols]
        import os
        if os.environ.get("KNOLOAD") and name.startswith("d"):
            return ring[s_]
        P.add("sp", lambda e: e.dma_start(out=dst.ap, in_=src), reads=[name], writes=[dst], dma="r%d" % s_)
        return ring[s_]

    def finish_rstd(bank, ncols, div, dst):
        k.act(tmpn[:, 0:ncols], pv_(bank, slice(None), slice(0, ncols)), AF.Sqrt, bias=eps_c, scale=1.0 / div)
        k.recip(dst, tmpn[:, 0:ncols])

    def prenorm(src, dst_bf, gain_grp, l, ncols, rst):
        for c in range(8):
            if c % 2 == 0:
                k.act(dst_bf[:, c, 0:ncols], src[:, c, 0:ncols], AF.Square)
            else:
                k.tt("pool", dst_bf[:, c, 0:ncols], src[:, c, 0:ncols], src[:, c, 0:ncols], ALU.mult)
        for c in range(8):
            k.mm(pv_(7, slice(None), slice(0, ncols)), ones_b.all(), dst_bf[:, c, 0:ncols], start=(c == 0), stop=(c == 7))
        finish_rstd(7, ncols, 1024.0, rst)
        for c in range(8):
            k.stt(dst_bf[:, c, 0:ncols], src[:, c, 0:ncols], pvc(l, gain_grp, c), rst, ALU.mult, ALU.mult)

    def post_evac(bank, fc, l, grp):
        import os
        ne = os.environ.get("KNOEVAC", "")
        if "d" not in ne:
            k.ts("dve", bigf[:, fc, :], pv_(bank), pvc(l, grp, fc), None, ALU.mult)
        if "a" not in ne:
            k.act(uT[:, fc, :], pv_(bank), AF.Square)

    def post_finish():
        for fc in range(8):
            k.mm(pv_(7), ones_b.all(), uT[:, fc, :], start=(fc == 0), stop=(fc == 7))
        finish_rstd(7, TB, 1024.0, rstd.all())
        for fc in range(8):
            k.tt("pool", bigf[:, fc, :], bigf[:, fc, :], rstd.all(), ALU.mult)
            k.tt("pool" if fc % 2 else "dve", hT[:, fc, :], hT[:, fc, :], bigf[:, fc, :], ALU.add)

    def fm_chunk(wbuf, col0, bank=None, rhs=None, ncols=TB):
        b_ = ps(bank)
        for kc in range(8):
            k.mm(pv_(b_, slice(None), slice(0, ncols)), wbuf[:, kc, col0:col0 + 128],
                 (rhs if rhs is not None else uT)[:, kc, 0:ncols], start=(kc == 0), stop=(kc == 7))
        return b_


    for b_ in "abcm":
        if b_ not in en:
            k.memset("pool", yT[b_].all(), 0.0)
    S0 = slice(None)
    CI = (IDX_C := (64 ** -0.5) * (8 ** -0.5))

    def mem_setup(s):
        P.tag = 'memsetup'
        for nb in range(2):
            k.dma("sp", xt[:, nb, :], mem_d[s, 128 * nb:128 * nb + 128, :], "xin")
        sync_group([xt[:, 0:2, :]], "xin")
        for c in range(8):
            b_ = ps()
            for nb in range(2):
                k.tr(pv_(b_, S0, slice(128 * nb, 128 * nb + 128)), xt[:, nb, 128 * c:128 * c + 128], ident_f)
            k.cp("act" if c % 2 else "dve", memT[:, c, :], pv_(b_, S0, slice(0, NMEM)))
        for l in range(L):
            prenorm(memT, unT, 4, l, NMEM, rstd[:, 0:NMEM])
            w = load_unit("mem_%d" % l)
            for hp in range(2):
                b_ = fm_chunk(w, 128 * hp, rhs=unT, ncols=NMEM)
                k.cp("act", mk2T[l][:, hp, :], pv_(b_, S0, slice(0, NMEM)))
            for nb in range(2):
                b_ = ps()
                for kc in range(8):
                    k.mm(pv_(b_, S0, slice(0, 256)), unT[:, kc, 128 * nb:128 * nb + 128], w[:, kc, 256:512],
                         start=(kc == 0), stop=(kc == 7))
                src3 = View(psb[b_][:, 0:256].rearrange("p (h d) -> p h d", d=64), [("ps", b_)])
                k.cp("dve", mvdup[l][:, nb, :, 0:64], src3)
                k.cp("act", mvdup[l][:, nb, :, 64:128], src3)

    def load_block(s, blk):
        P.tag = 'load'
        for tt in range(4):
            r0 = blk * TB + 128 * tt
            k.dma("sp", xt[:, tt, :], x_d[s, r0:r0 + 128, :], "xin")
        sync_group([xt.all()], "xin")
        for c in range(8):
            b_ = ps()
            for tt in range(4):
                k.tr(pv_(b_, S0, slice(128 * tt, 128 * tt + 128)), xt[:, tt, 128 * c:128 * c + 128], ident_f)
            k.cp("act" if c % 2 else "dve", hT[:, c, :], pv_(b_))

    outkeys = []

    def store_block(s, blk):
        P.tag = 'store'
        for tt in range(4):
            b0, b1 = ps(), ps()
            for c in range(8):
                bb = b0 if c < 4 else b1
                k.tr(pv_(bb, S0, slice(128 * (c % 4), 128 * (c % 4) + 128)), hT[:, c, 128 * tt:128 * tt + 128], ident_f)
            k.cp("dve", xt[:, tt, 0:512], pv_(b0))
            k.cp("act", xt[:, tt, 512:1024], pv_(b1))
            r0 = blk * TB + 128 * tt
            key = "out%d" % len(outkeys)
            outkeys.append(key)
            k.dma("sp", out_d[s, r0:r0 + 128, :], xt[:, tt, :], "xout%d" % tt, writes=[key])

    def mix(s, blk, l):
        first = (blk == 0)
        W = 16 + TB
        lo_, up_ = slice(0, 64), slice(64, 128)
        P.tag = 'mix_norm'
        prenorm(hT, uT, 0, l, TB, rstd.all())
        P.tag = 'tok'
        wT = load_unit("inT_%d" % l, 8, 328)
        for tt in range(4):
            g = blk * 4 + tt
            b_ = ps()
            for kc in range(8):
                k.mm(pv_(b_, S0, slice(0, 328)), uT[:, kc, 128 * tt:128 * tt + 128], wT[:, kc, 0:328],
                     start=(kc == 0), stop=(kc == 7))
            k.cp("act", itok[:, tt, :], pv_(b_, S0, slice(0, 256)))
            for par in range(2):
                src3 = View(psb[b_][:, 0:256].rearrange("p (hp q d) -> p hp q d", q=2, d=64)[:, :, par, :],
                            [("ps", b_)])
                k.cp("dve", ipad[:, tt, slice(par, 4, 2), 64 * par:64 * par + 64], src3)
            k.cp("dve", vdup[l][:, g, 0:64], pv_(b_, S0, slice(256, 320)))
            k.cp("act", vdup[l][:, g, 64:128], pv_(b_, S0, slice(256, 320)))
            k.act(absw[:, tt, :], pv_(b_, S0, slice(320, 328)), AF.Abs, scale=IDX_C)
            k.act(sgn[:, tt, :], pv_(b_, S0, slice(320, 328)), AF.Sign)
        P.tag = 'gz'
        w2 = load_unit("in2_%d" % l)
        for hp in range(2):
            b_ = fm_chunk(w2, 128 * hp)
            k.act(gs[:, hp, :], pv_(b_), AF.Silu)
        for ci in range(2):
            if first:
                k.memset("pool", zbuf[:, ci, 0:16], 0.0)
            else:
                k.cp("pool", zbuf[:, ci, 0:16], halo[:, l, ci, :])
            b_ = fm_chunk(w2, 256 + 128 * ci)
            k.cp("act", zbuf[:, ci, 16:W], pv_(b_))
            k.cp("pool", halo[:, l, ci, :], zbuf[:, ci, TB:W])
        P.tag = 'hgrn'
        w0 = load_unit("in0_%d" % l)
        w1 = load_unit("in1_%d" % l)
        for h in range(4):
            if "a" not in en:
                break
            hh = h % 2
            bq = fm_chunk(w0, 128 * h)
            bf = fm_chunk(w1, 128 * h)
            lb, oml, noml = lbt[:, l, 0, h:h + 1], lbt[:, l, 1, h:h + 1], lbt[:, l, 2, h:h + 1]
            k.act(hA.all(), pv_(bf), AF.Sigmoid)
            k.act(hC.all(), hA.all(), AF.Identity, bias=oml, scale=noml)
            k.ts("dve", hA.all(), hA.all(), oml, lb, ALU.mult, ALU.add)
            k.act(hA.all(), hA.all(), AF.Ln)
            k.scan(hB.all(), c_("scanmask", 512), hA.all(), 0.0, ALU.mult, ALU.add)
            for c in range(8):
                cs = slice(64 * c, 64 * c + 64)
                k.ts("dve", hA2[:, cs], hB[:, cs], hB[:, 64 * c + 63:64 * c + 64], None, ALU.subtract)
            k.act(hE2.all(), hA2.all(), AF.Exp, scale=-1.0)
            k.tt("dve", hF.all(), hC.all(), hE2.all(), ALU.mult)
            k.act(hE.all(), hB.all(), AF.Exp)
            k.cp("dve", dcol.all(), View(hE.h[:, 63:512:64], hE.all().keys))
            bt = ps()
            for p_ in range(4):
                k.tr(pv_(bt, S0, slice(128 * p_, 128 * p_ + 128)), hF[:, 128 * p_:128 * p_ + 128], ident_f)
            src3 = psb[bt][:, :].rearrange("p (a q) -> p a q", q=128)
            k.cp("dve", ktok[lo_, :, 0, :], View(src3[0:64], [("ps", bt)]))
            k.cp("act", ktok[up_, :, 1, :], View(src3[64:128], [("ps", bt)]))
            bu = ps()
            for c in range(8):
                k.mm(pv_(bu, S0, slice(64 * c, 64 * c + 64)), ktok[:, c // 2, c % 2, :],
                     itok[:, c // 2, 64 * h:64 * h + 64])
            S = Sst[:, l, h, :]
            if first:
                k.memset("dve", S, 0.0)
            for c in range(8):
                k.cp("dve", Spad[hh][:, c, 64 * hh:64 * hh + 64], S)
                k.stt(S, S, dcol[:, c:c + 1], pv_(bu, S0, slice(64 * c, 64 * c + 64)), ALU.mult, ALU.add)
            k.act(hQ.all(), pv_(bq), AF.Silu)
            k.tt("pool", qS[hh].all(), hQ.all(), hE.all(), ALU.mult)
            for c in range(8):
                cs = slice(64 * c, 64 * c + 64)
                k.ts("dve", hA[:, cs], hB[:, cs], hB[:, 64 * c + 31:64 * c + 32], None, ALU.subtract)
            k.act(hE2.all(), hA.all(), AF.Exp)
            k.tt("dve", Qp.all(), hQ.all(), hE2.all(), ALU.mult)
            k.act(hE.all(), hA.all(), AF.Exp, scale=-1.0)
            k.tt("pool", Kp.all(), hC.all(), hE.all(), ALU.mult)
            ba = ps()
            for p_ in range(4):
                rg = slice(128 * p_, 128 * p_ + 128)
                k.mm(pv_(ba, S0, rg), Kp[:, rg], Qp[:, rg])
            k.tt("dve", attm[hh].all(), pv_(ba), c_("trimask", 512), ALU.mult)
            if hh == 1:
                hp = h // 2
                for p_ in range(4):
                    rg = slice(128 * p_, 128 * p_ + 128)
                    k.mm(pv_(6, S0, rg), ipad[:, p_, 2 * hp, :], attm[0][:, rg], start=True, stop=False)
                    k.mm(pv_(6, S0, rg), ipad[:, p_, 2 * hp + 1, :], attm[1][:, rg], start=False, stop=False)
                    for c in (2 * p_, 2 * p_ + 1):
                        for q in range(2):
                            cs = slice(64 * c, 64 * c + 64)
                            k.mm(pv_(6, S0, cs), Spad[q][:, c, :], qS[q][:, cs], start=False,
                                 stop=(c == 2 * p_ + 1 and q == 1))
                sq = sqr[0]
                k.act(sq.all(), pv_(6), AF.Square)
                bn = ps()
                k.mm(pv_(bn), bdones_b.all(), sq.all())
                k.act(tmpn.all(), pv_(bn), AF.Sqrt, bias=eps_c, scale=1.0 / 64)
                k.recip(oT.all(), tmpn.all())
                k.stt(oT.all(), pv_(6), pv_hgn(l, hp), oT.all(), ALU.mult, ALU.mult)
                k.tt("pool", yT["a"][:, hp, :], oT.all(), gs[:, hp, :], ALU.mult)
        P.tag = 'pool'
        if "b" in en:
            for ci in range(2):
                if ci == 0:
                    k.tt("pool", sfin[lo_, 1:W], zbuf[lo_, ci, 1:W], zbuf[lo_, ci, 0:W - 1], ALU.add)
                    k.tt("pool", s2b[up_, 1:W], zbuf[up_, ci, 1:W], zbuf[up_, ci, 0:W - 1], ALU.add)
                    k.tt("pool", sfin[up_, 3:W], s2b[up_, 3:W], s2b[up_, 1:W - 2], ALU.add)
                else:
                    k.tt("pool", s2b[:, 1:W], zbuf[:, ci, 1:W], zbuf[:, ci, 0:W - 1], ALU.add)
                    k.tt("pool", s4b[:, 3:W], s2b[:, 3:W], s2b[:, 1:W - 2], ALU.add)
                    k.tt("pool", sfin[lo_, 7:W], s4b[lo_, 7:W], s4b[lo_, 3:W - 4], ALU.add)
                    k.tt("pool", s8b[up_, 7:W], s4b[up_, 7:W], s4b[up_, 3:W - 4], ALU.add)
                    k.tt("pool", sfin[up_, 15:W], s8b[up_, 15:W], s8b[up_, 7:W - 8], ALU.add)
                o_ = C_OFF["invw"] + ci
                k.stt(pooled_b[:, ci, :], sfin[:, 16:W], cst[:, o_:o_ + 1], zbuf[:, ci, 16:W], ALU.mult, ALU.subtract)
                if first:
                    o2 = C_OFF["invcnt"] + 16 * ci
                    k.tt("dve", sfin[:, 0:16], sfin[:, 16:32], cst[:, o2:o2 + 16], ALU.mult)
                    k.tt("dve", pooled_b[:, ci, 0:16], sfin[:, 0:16], zbuf[:, ci, 16:32], ALU.subtract)
                b_ = ps()
                k.mm(pv_(b_), poolw_b[:, l, ci, :], pooled_b[:, ci, :])
                k.act(yT["b"][:, ci, :], pv_(b_), AF.Identity, scale=pv_psc(l, ci))
        P.tag = 'dsa_in'
        w3 = load_unit("in3_%d" % l)
        for hp in range(2):
            b_ = fm_chunk(w3, 128 * hp)
            k.act(sqpad[lo_, 2 * hp, :], pv_(b_, lo_), AF.Copy, scale=0.125)
            k.act(sqpad[up_, 2 * hp + 1, :], pv_(b_, up_), AF.Copy, scale=0.125)
        cb = slice(blk * TB, blk * TB + TB)
        b_ = fm_chunk(w3, 256)
        k.cp("dve", skdup[l][:, cb], pv_(b_))
        b_ = fm_chunk(w3, 384)
        k.cp("act", ikdup[l][:, cb], pv_(b_))
        w4 = load_unit("in4_%d" % l)
        for hp in range(4):
            b_ = fm_chunk(w4, 128 * hp)
            k.cp("dve", iqpad[lo_, 2 * hp, :], pv_(b_, lo_))
            k.cp("act", iqpad[up_, 2 * hp + 1, :], pv_(b_, up_))
        w5 = load_unit("in5_%d" % l, 8, 256)
        for hp in range(2):
            b_ = fm_chunk(w5, 128 * hp)
            k.act(mqpad[lo_, 2 * hp, :], pv_(b_, lo_), AF.Copy, scale=0.125)
            k.act(mqpad[up_, 2 * hp + 1, :], pv_(b_, up_), AF.Copy, scale=0.125)
        P.tag = 'dsa'
        def mem_attn():
            P.tag = 'mem'
            if "m" not in en:
                return
            for h in range(4):
                hh, hp = h % 2, h // 2
                for nb in range(2):
                    b_ = ps()
                    k.mm(pv_(b_), mk2T[l][:, hp, 128 * nb:128 * nb + 128], mqpad[:, h, :])
                    p_t = pT[nb]
                    k.act(p_t.all(), pv_(b_), AF.Exp)
                    k.mm(pv_(6), mvdup[l][:, nb, h, :], p_t.all(), start=(nb == 0), stop=(nb == 1))
                for nb in range(2):
                    k.mm(pv_(7), ones_b.all(), pT[nb].all(), start=(nb == 0), stop=(nb == 1))
                rows = lo_ if hh == 0 else up_
                k.recip(rec[rows, :], pv_(7, rows))
                k.tt("dve", yT["m"][rows, hp, :], pv_(6, rows), rec[rows, :], ALU.mult)
            P.tag = 'dsa'

        if "c" in en:
            dsa_score(l, blk, 0)
            dsa_bisect(l, blk, 0)
            mem_attn()
            for tt in range(1, 4):
                dsa_score(l, blk, tt)
                dsa_bisect(l, blk, tt)
                dsa_attend(l, blk, tt - 1)
            dsa_attend(l, blk, 3)
        else:
            mem_attn()
        P.tag = 'mem'
        P.tag = 'gates'
        for half in range(2):
            src = scr["br%d_%d" % (half, l)]
            P.add("sp", (lambda src=src: (lambda e: e.dma_start(out=wbr.all().ap, in_=src)))(),
                  reads=["br%d_%d" % (half, l)], writes=[wbr.all()], dma="wbr")
            for bi, b_name in enumerate("abcm"):
                wg = load_unit("g%d_%d_%d" % (bi, half, l))
                for fq in range(4):
                    fc = 4 * half + fq
                    bg = fm_chunk(wg, 128 * fq)
                    sg_ = sig[fq % 2]
                    k.act(sg_.all(), pv_(bg), AF.Sigmoid)
                    bb = ps()
                    for kc in range(2):
                        k.mm(pv_(bb), wbr[:, 2 * bi + kc, 128 * fq:128 * fq + 128], yT[b_name][:, kc, :],
                             start=(kc == 0), stop=(kc == 1))
                    if bi == 0:
                        k.tt("dve", bigf[:, fc, :], pv_(bb), sg_.all(), ALU.mult)
                    else:
                        t_ = tb[fq % 2]
                        k.tt("dve", t_.all(), pv_(bb), sg_.all(), ALU.mult)
                        if bi < 3:
                            k.tt("pool", bigf[:, fc, :], bigf[:, fc, :], t_.all(), ALU.add)
                        else:
                            k.tt("pool", merged_b[:, fc, :], bigf[:, fc, :], t_.all(), ALU.add)
        P.tag = 'wout'
        for half in range(2):
            wo = load_unit("o%d_%d" % (half, l))
            for fq in range(4):
                fc = 4 * half + fq
                b_ = fm_chunk(wo, 128 * fq, rhs=merged_b)
                post_evac(b_, fc, l, 1)
        post_finish()

    def dsa_score(l, blk, tt):
        g = blk * 4 + tt
        par = g % 2
        N = (g + 1) * 128
        qs = slice(128 * tt, 128 * tt + 128)
        sc = score[par]
        if g < 2:
            return
        for h in range(8):
            k.ts("dve", Dh[:, h, :], ident_b.all(), sgn[:, tt, h:h + 1], None, ALU.mult)
        npiece = (N + 511) // 512
        items = [(j, h) for j in range(npiece) for h in range(8)]
        bl_of = {}

        def geo(j):
            w_ = min(512, N - 512 * j)
            return slice(512 * j, 512 * j + w_), slice(0, w_)

        def logits(i):
            j, h = items[i]
            ks_, ws_ = geo(j)
            bl_of[i] = ps()
            k.mm(pv_(bl_of[i], S0, ws_), iqpad[:, h, qs], ikdup[l][:, ks_])

        LA = 3
        for i in range(min(LA, len(items))):
            logits(i)
        for i, (j, h) in enumerate(items):
            ks_, ws_ = geo(j)
            bsc = 6 + (j % 2)
            r_ = rh[i % 3]
            k.act(r_[:, ws_], pv_(bl_of[i], S0, ws_), AF.Relu, scale=absw[:, tt, h:h + 1])
            if i + LA < len(items):
                logits(i + LA)
            k.mm(pv_(bsc, S0, ws_), Dh[:, h, :], r_[:, ws_], start=(h == 0), stop=(h == 7))
            if h == 7:
                k.cp("act", sc[:, ks_], pv_(bsc, S0, ws_))

    def dsa_bisect(l, blk, tt):
        g = blk * 4 + tt
        par = g % 2
        N = (g + 1) * 128
        sc, mb = score[par], mbias[par]
        if g < 2:
            if N > 128:
                k.memset("pool", mb[:, 0:N - 128], 0.0)
            k.cp("dve", mb[:, N - 128:N], c_("mbdiag", 128))
            return
        A_, lo_c, mid_c, cnt_c, inc_c = [cols[:, par, i:i + 1] for i in range(5)]
        hw = cols[:, par, 8:8 + NBIS]
        k.reduce(A_, sc[:, 0:N], ALU.max, absval=True)
        k.tt("pool", sc[:, N - 128:N], sc[:, N - 128:N], c_("negadm", 128), ALU.add)
        k.ts("dve", lo_c, A_, -1.0, None, ALU.mult)
        k.ts("dve", hw, c_("pow2", NBIS), A_, None, ALU.mult)
        for it in range(NBIS):
            s_i = cols[:, par, 8 + it:9 + it]
            k.ts("dve", mid_c, lo_c, s_i, None, ALU.add)
            k.ts("dve", mb[:, 0:N], sc[:, 0:N], mid_c, None, ALU.is_ge, ALU.add, accum=cnt_c)
            k.ts("dve", inc_c, cnt_c, 256.0, s_i, ALU.is_ge, ALU.mult)
            k.tt("dve", lo_c, lo_c, inc_c, ALU.add)
        k.ts("dve", mb[:, 0:N], sc[:, 0:N], lo_c, -30000.0, ALU.is_lt, ALU.mult)

    def dsa_attend(l, blk, tt):
        g = blk * 4 + tt
        par = g % 2
        qs = slice(128 * tt, 128 * tt + 128)
        lo_, up_ = slice(0, 64), slice(64, 128)
        mb = mbias[par]
        qk_bank = {}

        def qk(kb):
            ksl = slice(128 * kb, 128 * kb + 128)
            b_ = ps()
            qk_bank[kb] = b_
            for h in range(4):
                hs = slice(128 * h, 128 * h + 128)
                k.mm(pv_(b_, S0, hs), mb[:, ksl], ident_b.all(), start=True, stop=False)
                k.mm(pv_(b_, S0, hs), skdup[l][:, ksl], sqpad[:, h, qs], start=False, stop=True)

        qk(0)
        for kb in range(g + 1):
            p_t = pT[kb % 2]
            k.act(p_t.all(), pv_(qk_bank[kb]), AF.Exp)
            if kb + 1 <= g:
                qk(kb + 1)
            k.mm(pv_(6), vdup[l][:, kb, :], p_t.all(), start=(kb == 0), stop=(kb == g))
            k.mm(pv_(7), ones_b.all(), p_t.all(), start=(kb == 0), stop=(kb == g))
        k.recip(rec.all(), pv_(7))
        for h in range(4):
            rows = lo_ if h % 2 == 0 else up_
            hs = slice(128 * h, 128 * h + 128)
            k.tt("dve", yT["c"][rows, h // 2, qs], pv_(6, rows, hs), rec[rows, hs], ALU.mult)

    def ffn(l):
        import os
        kf = int(os.environ.get("KF", "9"))
        P.tag = 'ffn_norm'
        prenorm(hT, uT, 2, l, TB, rstd.all())
        P.tag = 'ffn_gu'
        if kf < 2:
            return
        for i in range(11):
            w = load_unit("gu%d_%d" % (i, l))
            for q in range(2):
                j = 2 * i + q
                bg = fm_chunk(w, 128 * q)
                bu = fm_chunk(w, 256 + 128 * q)
                s_ = sgt[j % 2]
                k.act(s_.all(), pv_(bg), AF.Silu)
                k.tt("dve", actT[:, j, :], pv_(bu), s_.all(), ALU.mult)
        if kf < 3:
            return
        P.tag = 'ffn_down'
        for half in range(2):
            wd = [load_unit("d%d_%d_%d" % (jg, half, l), min(8, 22 - 8 * jg), 512) for jg in range(3)]
            for fq in range(4):
                fc = 4 * half + fq
                b_ = ps()
                for j in range(22):
                    k.mm(pv_(b_), wd[j // 8][:, j % 8, 128 * fq:128 * fq + 128], actT[:, j, :],
                         start=(j == 0), stop=(j == 21))
                post_evac(b_, fc, l, 3)
        if kf < 4:
            return
        post_finish()

    for s in range(nseq):
        if "m" in en:
            mem_setup(s)
        for blk in range(NBLK):
            load_block(s, blk)
            import os
            dbg = os.environ.get("KDBG", "mf")
            for l in range(L):
                if "m" in dbg:
                    mix(s, blk, l)
                if "f" in dbg:
                    ffn(l)
            store_block(s, blk)
    P.add("sp", lambda e: e.nop(), reads=outkeys)
    P.emit()
    return nc, P


_CACHE = {}


def run(inputs, nseq, T, L, ncores, en=("a", "b", "c", "m"), trace=False):
    key = (nseq, T, L, tuple(en))
    if key not in _CACHE:
        _CACHE[key] = build(nseq, T, L, en)[0]
    nc = _CACHE[key]
    f = lambda a: np.ascontiguousarray(np.asarray(a), dtype=np.float32)
    x = f(inputs["x"])
    mem = f(inputs["mem"])
    shared = {
        "consts": CONSTS,
        "pvec": _mk_pvec(inputs, L),
        "poolw": _mk_poolw(f(inputs["pool_w"]), L),
        "w_in": f(inputs["w_in"])[:L],
        "w_gate": f(inputs["w_gate"])[:L],
        "w_branch": f(inputs["w_branch"])[:L],
        "w_out": f(inputs["w_out"])[:L],
        "w_mem_kv": f(inputs["w_mem_kv"])[:L],
        "w_ffn_gate_up": f(inputs["w_ffn_gate_up"])[:L],
        "w_ffn_down": f(inputs["w_ffn_down"])[:L],
    }
    in_maps = []
    for c in range(ncores):
        m = dict(shared)
        m["x"] = np.ascontiguousarray(x[c * nseq:(c + 1) * nseq, :T])
        m["mem"] = np.ascontiguousarray(mem[c * nseq:(c + 1) * nseq])
        in_maps.append(m)
    res = run_bass_kernel_spmd(nc, in_maps, core_ids=list(range(ncores)), trace=trace)
    out = np.concatenate([np.asarray(r["out"]) for r in res.results], axis=0)
    return out.astype(np.float32), res


def kernel(**inputs):
    out, _ = run(inputs, 2, 2048, 2, 8)
    return out
```

```python
import numpy as np
import concourse.bass as bass
import concourse.mybir as mybir
from concourse.bass_utils import run_bass_kernel_spmd

F32 = mybir.dt.float32
BF16 = mybir.dt.bfloat16
AF = mybir.ActivationFunctionType
ALU = mybir.AluOpType
AX = mybir.AxisListType

D = 1024
TB = 512
DFF = 2816
NIN = 3016
NMEM = 256
EPS = 1e-6
PAGE = 64
NBIS = 16
DVE_INORDER = False


class View:
    __slots__ = ("ap", "keys")

    def __init__(self, ap, keys):
        self.ap = ap
        self.keys = keys


class Buf:
    def __init__(self, nc, name, shape, dtype, off):
        self.shape = list(shape)
        self.esz = 4 if dtype == F32 else 2
        self.off = off
        self.h = nc.alloc_sbuf_tensor_at(name, self.shape, dtype, offset=off)
        self.strides = []
        s = 1
        for d in reversed(self.shape[1:]):
            self.strides.insert(0, s)
            s *= d
        self.nelem = s
        self.nbytes = s * self.esz

    def __getitem__(self, idx):
        if not isinstance(idx, tuple):
            idx = (idx,)
        lo = 0
        hi = 0
        for k, st in enumerate(self.strides):
            dim = self.shape[k + 1]
            if k + 1 < len(idx):
                i = idx[k + 1]
                if isinstance(i, slice):
                    a = 0 if i.start is None else i.start
                    b = dim if i.stop is None else i.stop
                else:
                    a, b = i, i + 1
            else:
                a, b = 0, dim
            lo += a * st
            hi += (b - 1) * st
        hi += 1
        b0 = (self.off + lo * self.esz) // PAGE
        b1 = (self.off + hi * self.esz - 1) // PAGE
        return View(self.h[idx], [("sb", p) for p in range(b0, b1 + 1)])

    def all(self):
        return self[(slice(None),) * len(self.shape)]


class Prog:
    ENG = ("sp", "pe", "act", "dve", "pool")

    def __init__(self, nc):
        self.nc = nc
        self.ops = {e: [] for e in self.ENG}
        self.cnt = {}
        self.sem = {}
        self.res = {}
        self.known = {e: {} for e in self.ENG}
        self.epoch = {}
        self.nops = 0
        self.tag = ''
        self.tags = {e: [] for e in self.ENG}

    def _sem(self, key):
        if key not in self.sem:
            self.sem[key] = self.nc.alloc_semaphore("s_" + key)
            self.cnt[key] = 0

    def add(self, eng, fn, reads=(), writes=(), dma=None):
        deps = set()
        rk = []
        wk = []
        for v in reads:
            for key in (v.keys if isinstance(v, View) else [v]):
                if isinstance(key, tuple) and key[0] == "ps":
                    wk.append(key)
                else:
                    rk.append(key)
        for v in writes:
            wk.extend(v.keys if isinstance(v, View) else [v])
        for k in rk:
            r = self.res.get(k)
            if r is not None and r[0] is not None:
                deps.add(r[0])
        for k in wk:
            r = self.res.get(k)
            if r is not None:
                if r[0] is not None:
                    deps.add(r[0])
                deps.update(r[1])
        if dma:
            semkey = dma
        else:
            ep = self.epoch.get(eng, 0)
            semkey = "%s_%d" % (eng, ep)
            if self.cnt.get(semkey, 0) >= 4000:
                self.epoch[eng] = ep + 1
                semkey = "%s_%d" % (eng, ep + 1)
        self._sem(semkey)
        inc = 16 if dma else 1
        self.cnt[semkey] += inc
        tok = (semkey, self.cnt[semkey])
        waits = {}
        kn = self.known[eng]
        for (sk, v) in deps:
            if eng == "pe" and sk.startswith("pe_"):
                continue
            if eng == "dve" and sk.startswith("dve_") and DVE_INORDER:
                continue
            if kn.get(sk, 0) >= v:
                continue
            if waits.get(sk, 0) < v:
                waits[sk] = v
        for sk, v in waits.items():
            kn[sk] = v
        self.ops[eng].append((list(waits.items()), fn, semkey, inc))
        self.tags[eng].append(self.tag)
        self.nops += 1
        for k in rk:
            r = self.res.get(k)
            if r is None:
                self.res[k] = [None, [tok]]
            else:
                r[1].append(tok)
        for k in wk:
            self.res[k] = [tok, []]
        return tok

    def emit(self):
        nc = self.nc

        def mk(eng):
            def body(e):
                for waits, fn, semkey, inc in self.ops[eng]:
                    for sk, v in waits:
                        e.wait_ge(self.sem[sk], v)
                    fn(e).then_inc(self.sem[semkey], inc)
            return body

        with nc.Block() as block:
            block.sync(mk("sp"))
            block.tensor(mk("pe"))
            block.scalar(mk("act"))
            block.vector(mk("dve"))
            block.gpsimd(mk("pool"))


def _a(v):
    return v.ap if isinstance(v, View) else v


def _vs(*xs):
    return [x for x in xs if isinstance(x, View)]


class K:
    def __init__(self, P):
        self.P = P

    def mm(self, out, lhsT, rhs, start=True, stop=True):
        self.P.add("pe", lambda e: e.matmul(out.ap, lhsT.ap, rhs.ap, start=start, stop=stop),
                   reads=[lhsT, rhs], writes=[out])

    def tr(self, out, in_, ident):
        self.P.add("pe", lambda e: e.transpose(out.ap, in_.ap, ident.ap), reads=[in_, ident], writes=[out])

    def act(self, out, in_, func, bias=None, scale=None, accum=None):
        kw = {}
        if bias is not None:
            kw["bias"] = _a(bias)
        if scale is not None:
            kw["scale"] = _a(scale)
        if accum is not None:
            kw["accum_out"] = accum.ap
        self.P.add("act", lambda e: e.activation(out.ap, in_.ap, func, **kw),
                   reads=_vs(in_, bias, scale), writes=_vs(out, accum))

    def ts(self, eng, out, in0, s1, s2, op0, op1=None, accum=None):
        kw = {}
        if op1 is not None:
            kw["op1"] = op1
        if accum is not None:
            kw["accum_out"] = accum.ap
        self.P.add(eng, lambda e: e.tensor_scalar(out.ap, in0.ap, _a(s1), _a(s2), op0, **kw),
                   reads=_vs(in0, s1, s2), writes=_vs(out, accum))

    def tt(self, eng, out, in0, in1, op):
        self.P.add(eng, lambda e: e.tensor_tensor(out.ap, in0.ap, in1.ap, op), reads=[in0, in1], writes=[out])

    def stt(self, out, in0, s, in1, op0, op1):
        self.P.add("dve", lambda e: e.scalar_tensor_tensor(out.ap, in0.ap, _a(s), in1.ap, op0, op1),
                   reads=_vs(in0, s, in1), writes=[out])

    def cp(self, eng, out, in_):
        if eng == "act":
            self.P.add("act", lambda e: e.copy(out.ap, in_.ap), reads=[in_], writes=[out])
        else:
            self.P.add(eng, lambda e: e.tensor_copy(out.ap, in_.ap), reads=[in_], writes=[out])

    def recip(self, out, in_):
        self.P.add("dve", lambda e: e.reciprocal(out.ap, in_.ap), reads=[in_], writes=[out])

    def memset(self, eng, out, val):
        self.P.add(eng, lambda e: e.memset(out.ap, val), writes=[out])

    def reduce(self, out, in_, op, axis=AX.X, absval=None):
        self.P.add("dve", lambda e: e.tensor_reduce(out.ap, in_.ap, axis, op, apply_absolute_value=absval),
                   reads=[in_], writes=[out])

    def scan(self, out, d0, d1, init, op0, op1):
        self.P.add("dve", lambda e: e.tensor_tensor_scan(out.ap, d0.ap, d1.ap, _a(init), op0, op1),
                   reads=_vs(d0, d1, init), writes=[out])

    def dma(self, q, out, in_, sem, reads=(), writes=()):
        self.P.add(q, lambda e: e.dma_start(out=_a(out), in_=_a(in_)),
                   reads=list(reads) + _vs(in_), writes=list(writes) + _vs(out), dma=sem)


C_OFF = {}


def _mk_consts():
    cols = []

    def put(name, arr):
        C_OFF[name] = sum(a.shape[1] for a in cols)
        cols.append(arr.astype(np.float32))

    p = np.arange(128)
    put("ident", np.eye(128))
    put("ones", np.ones((128, 128)))
    bd = np.zeros((128, 128))
    bd[:64, :64] = 1
    bd[64:, 64:] = 1
    put("bdones", bd)
    t = np.arange(512)
    put("scanmask", np.tile((t % 64 != 0).astype(np.float32)[None, :], (128, 1)))
    s_ = p[:, None]
    t_ = np.arange(128)[None, :]
    tri = ((s_ // 64 == t_ // 64) & (t_ >= s_)).astype(np.float32)
    put("trimask", np.tile(tri, (1, 4)))
    inad = (p[:, None] < 64) & (np.arange(128)[None, :] >= 64)
    put("negadm", np.where(inad, -1e30, 0.0))
    put("mbdiag", np.where(inad, -30000.0, 0.0))
    ic = np.zeros((128, 2, 16))
    ws = [[2, 4], [8, 16]]
    for ci in range(2):
        for half in range(2):
            w = ws[ci][half]
            ic[half * 64:(half + 1) * 64, ci, :] = 1.0 / np.minimum(np.arange(16) + 1, w)[None, :]
    put("invcnt", ic.reshape(128, 32))
    iw = np.zeros((128, 2))
    for ci in range(2):
        for half in range(2):
            iw[half * 64:(half + 1) * 64, ci] = 1.0 / ws[ci][half]
    put("invw", iw)
    put("eps", np.full((128, 1), EPS))
    put("c256", np.full((128, 1), 256.0))
    put("pow2", np.tile((2.0 ** -np.arange(24))[None, :], (128, 1)))
    return np.ascontiguousarray(np.concatenate(cols, axis=1), dtype=np.float32)


CONSTS = _mk_consts()
NCONST = CONSTS.shape[1]
PV_PER_L = 44


def _mk_pvec(inp, L):
    cols = []
    for l in range(L):
        for nm in ("norm_mix_pre", "norm_mix_post", "norm_ffn_pre", "norm_ffn_post", "norm_mem"):
            cols.append(np.asarray(inp[nm][l]).reshape(8, 128).T)
        cols.append(np.asarray(inp["hgrn_norm"][l]).reshape(2, 128).T)
        cols.append(np.asarray(inp["pool_scale"][l]).reshape(2, 128).T)
    for l in range(L):
        cols.append(np.asarray(inp["lb_logits"][l]).reshape(4, 128).T)
    return np.ascontiguousarray(np.concatenate(cols, axis=1), dtype=np.float32)


def _mk_poolw(pw, L):
    out = np.zeros((L, 128, 2, 128), np.float32)
    for l in range(L):
        for g in range(4):
            ci, half = g // 2, g % 2
            out[l, half * 64:(half + 1) * 64, ci, half * 64:(half + 1) * 64] = pw[l, g]
    return out


def build(nseq, T, L=2, en=("a", "b", "c", "m")):
    nc = bass.Bass("TRN2", target_bir_lowering=False)
    NBLK = T // TB
    NT = T // 128
    P = Prog(nc)
    k = K(P)

    def din(name, shape):
        return nc.dram_tensor(name, list(shape), F32, kind="ExternalInput").ap()

    x_d = din("x", [nseq, T, D])
    mem_d = din("mem", [nseq, NMEM, D])
    consts_d = din("consts", [128, NCONST])
    NPV = PV_PER_L * L + 4 * L
    pvec_d = din("pvec", [128, NPV])
    poolw_d = din("poolw", [L, 128, 2, 128])
    w_in_d = din("w_in", [L, D, NIN])
    w_gate_d = din("w_gate", [L, 4, D, D])
    w_branch_d = din("w_branch", [L, 4, 256, D])
    w_out_d = din("w_out", [L, D, D])
    w_mem_d = din("w_mem_kv", [L, D, 512])
    w_gu_d = din("w_ffn_gate_up", [L, D, 2 * DFF])
    w_dn_d = din("w_ffn_down", [L, DFF, D])
    out_d = nc.dram_tensor("out", [nseq, T, D], F32, kind="ExternalOutput").ap()

    off = [((nc.sbuf_base + 63) // 64) * 64]
    sb_top = nc.sbuf_top

    def alloc(name, shape, dtype, at=None):
        esz = 4 if dtype == F32 else 2
        n = 1
        for d_ in shape[1:]:
            n *= d_
        nb = ((n * esz + 63) // 64) * 64
        if at is None:
            o = off[0]
            off[0] += nb
        else:
            o = at
        assert o + nb <= sb_top, (name, o, nb, sb_top)
        return Buf(nc, name, shape, dtype, o)

    cst = alloc("cst", [128, NCONST], F32)
    pv = alloc("pv", [128, NPV], F32)
    lbt = alloc("lbt", [128, L, 3, 4], F32)
    ident_b = alloc("ident_b", [128, 128], BF16)
    ones_b = alloc("ones_b", [128, 128], BF16)
    bdones_b = alloc("bdones_b", [128, 128], BF16)
    poolw_b = alloc("poolw_b", [128, L, 2, 128], BF16)
    hT = alloc("hT", [128, 8, TB], F32)
    uT = alloc("uT", [128, 8, TB], BF16)
    rstd = alloc("rstd", [128, TB], F32)
    bigf = alloc("bigf", [128, 8, TB], F32)
    xt = Buf(nc, "xt", [128, 4, D], F32, bigf.off)
    merged_b = alloc("merged_b", [128, 8, TB], BF16)
    ring = [alloc("ring%d" % i, [128, 8, 512], BF16) for i in range(4)]
    wbr = alloc("wbr", [128, 8, 512], BF16)
    yT = {b_: alloc("yT_" + b_, [128, 2, TB], BF16) for b_ in "abcm"}
    ikdup = [alloc("ikdup%d" % l, [128, T], BF16) for l in range(L)]
    skdup = [alloc("skdup%d" % l, [128, T], BF16) for l in range(L)]
    vdup = [alloc("vdup%d" % l, [128, NT, 128], BF16) for l in range(L)]
    Sst = alloc("Sst", [128, L, 4, 64], F32)
    halo = alloc("halo", [128, L, 2, 16], F32)
    mk2T = [alloc("mk2T%d" % l, [128, 2, NMEM], BF16) for l in range(L)]
    mvdup = [alloc("mvdup%d" % l, [128, 2, 4, 128], BF16) for l in range(L)]
    sqr = [alloc("sqr%d" % i, [128, TB], BF16) for i in range(2)]
    tmpn = alloc("tmpn", [128, TB], F32)
    zbuf = alloc("zbuf", [128, 2, 16 + TB], F32)
    sqpad = alloc("sqpad", [128, 4, TB], BF16)
    iqpad = alloc("iqpad", [128, 8, TB], BF16)
    mqpad = alloc("mqpad", [128, 4, TB], BF16)
    absw = alloc("absw", [128, 4, 8], F32)
    sgn = alloc("sgn", [128, 4, 8], F32)
    ipad = alloc("ipad", [128, 4, 4, 128], BF16)
    cols = alloc("cols", [128, 2, 32], F32)
    Spad = [alloc("Spad%d" % i, [128, 8, 128], BF16) for i in range(2)]
    ktok = alloc("ktok", [128, 4, 2, 128], BF16)
    arena0 = off[0]

    def phase():
        off[0] = arena0

    phase()
    itok = alloc("itok", [128, 4, 256], BF16)
    gs = alloc("gs", [128, 2, TB], BF16)
    hA = alloc("hA", [128, TB], F32)
    hB = alloc("hB", [128, TB], F32)
    hC = alloc("hC", [128, TB], F32)
    hQ = alloc("hQ", [128, TB], F32)
    hE = alloc("hE", [128, TB], F32)
    hF = alloc("hF", [128, TB], F32)
    Sall = alloc("Sall", [128, 8, 64], F32)
    hA2 = alloc("hA2", [128, TB], F32)
    hE2 = alloc("hE2", [128, TB], F32)
    Qp = alloc("Qp", [128, TB], BF16)
    Kp = alloc("Kp", [128, TB], BF16)
    qS = [alloc("qS%d" % i, [128, TB], BF16) for i in range(2)]
    attm = [alloc("attm%d" % i, [128, TB], BF16) for i in range(2)]
    dcol = alloc("dcol", [128, 8], F32)
    oT = alloc("oT", [128, TB], F32)
    end_h = off[0]
    phase()
    s2b = alloc("s2b", [128, 16 + TB], F32)
    s4b = alloc("s4b", [128, 16 + TB], F32)
    s8b = alloc("s8b", [128, 16 + TB], F32)
    sfin = alloc("sfin", [128, 16 + TB], F32)
    pooled_b = alloc("pooled_b", [128, 2, TB], BF16)
    end_p = off[0]
    phase()
    Dh = alloc("Dh", [128, 8, 128], BF16)
    rh = [alloc("rh%d" % i, [128, 512], BF16) for i in range(3)]
    score = [alloc("score%d" % i, [128, T], F32) for i in range(2)]
    mbias = [alloc("mbias%d" % i, [128, T], BF16) for i in range(2)]
    pT = [alloc("pT%d" % i, [128, 512], BF16) for i in range(2)]
    rec = alloc("rec", [128, 512], F32)
    end_d = off[0]
    phase()
    memT = alloc("memT", [128, 8, NMEM], F32)
    unT = alloc("unT", [128, 8, NMEM], BF16)
    end_m = off[0]
    phase()
    sig = [alloc("sig%d" % i, [128, TB], BF16) for i in range(2)]
    tb = [alloc("tb%d" % i, [128, TB], F32) for i in range(2)]
    end_g = off[0]
    phase()
    actT = alloc("actT", [128, 22, TB], BF16)
    sgt = [alloc("sgt%d" % i, [128, TB], BF16) for i in range(2)]
    end_f = off[0]
    print('SBUF use', arena0, max(end_h, end_p, end_d, end_m, end_g, end_f), sb_top)
    assert max(end_h, end_p, end_d, end_m, end_g, end_f) <= sb_top

    psb = [nc.alloc_psum_tensor("ps%d" % i, [128, 512], F32) for i in range(8)]
    rr = [0]

    def ps(bank=None):
        if bank is None:
            bank = rr[0] % 6
            rr[0] += 1
        return bank

    def pv_(bank, *idx):
        if not idx:
            idx = (slice(None), slice(None))
        return View(psb[bank][idx], [("ps", bank)])

    def c_(name, n, rows=slice(None)):
        o = C_OFF[name]
        return cst[rows, o:o + n]

    ident_f = c_("ident", 128)
    eps_c = c_("eps", 1)

    def pvc(l, grp, j):
        o = PV_PER_L * l + grp * 8 + j
        return pv[:, o:o + 1]

    def pv_hgn(l, j):
        o = PV_PER_L * l + 40 + j
        return pv[:, o:o + 1]

    def pv_psc(l, j):
        o = PV_PER_L * l + 42 + j
        return pv[:, o:o + 1]

    scr = {}

    def mk_scr(name, nk, ncols):
        scr[name] = nc.dram_tensor("scr_" + name, [128, nk, ncols], BF16, kind="Internal").ap()

    cast_i = [0]

    def cast(name, c0, src, sem):
        import os
        if os.environ.get("KNOCAST"):
            return
        n = src.shape[-1]
        cast_i[0] += 1
        dst = scr[name][:, 0:src.shape[1], c0:c0 + n]
        P.add("pool", lambda e: e.dma_start(out=dst, in_=src), reads=[], writes=["cast%d" % cast_i[0]], dma=sem)

    def finish_cast(names, sem):
        if sem not in P.cnt:
            return
        tok = (sem, P.cnt[sem])
        for nm in names:
            P.res[nm] = [tok, []]

    def rows_kc(ap2d):
        return ap2d.rearrange("(kc p) n -> p kc n", p=128)

    def sync_group(views, sem):
        tok = (sem, P.cnt[sem])
        for v in views:
            for key in v.keys:
                P.res[key] = [tok, []]

    P.tag = 'setup'
    k.dma("sp", cst.all(), consts_d, "init")
    k.dma("sp", pv.all(), pvec_d, "init")
    sync_group([cst.all(), pv.all()], "init")
    k.cp("dve", ident_b.all(), ident_f)
    k.cp("dve", ones_b.all(), c_("ones", 128))
    k.cp("dve", bdones_b.all(), c_("bdones", 128))
    for buf in (sqpad, iqpad, mqpad, ipad, ktok, Spad[0], Spad[1]):
        k.memset("pool", buf.all(), 0.0)
    lo_ = PV_PER_L * L
    for l in range(L):
        if l == 0:
            k.memset("dve", lbt[:, l, 0, :], 0.0)
        else:
            assert L == 2
            k.tt("dve", lbt[:, l, 1, :], pv[:, lo_ + 4:lo_ + 8], pv[:, lo_:lo_ + 4], ALU.subtract)
            k.act(lbt[:, l, 0, :], lbt[:, l, 1, :], AF.Sigmoid)
        k.ts("dve", lbt[:, l, 1, :], lbt[:, l, 0, :], -1.0, 1.0, ALU.mult, ALU.add)
        k.ts("dve", lbt[:, l, 2, :], lbt[:, l, 1, :], -1.0, None, ALU.mult)
    for l in range(L):
        k.dma("sp", tmpn[:, 0:256], poolw_d[l].rearrange("p a b -> p (a b)"), "init2")
        k.cp("dve", poolw_b[:, l, :, :], View(tmpn.h[:, 0:256].rearrange("p (a b) -> p a b", b=128), tmpn[:, 0:256].keys))

    stage = [Buf(nc, "stage0", [128, 8, 512], F32, bigf.off), Buf(nc, "stage1", [128, 8, 512], F32, hT.off),
             Buf(nc, "stage2", [128, 8, 512], F32, arena0), Buf(nc, "stage3", [128, 8, 512], F32, arena0 + 16384)]
    cast_n = [0]

    def cast_unit(nm, nk, ncols, pieces):
        mk_scr(nm, nk, ncols)
        i = cast_n[0]
        cast_n[0] += 1
        stg = stage[i % 4]
        for (k0, c0, s_ap) in pieces:
            nkk, n = s_ap.shape[1], s_ap.shape[2]
            k.dma("sp", stg[:, k0:k0 + nkk, c0:c0 + n], s_ap, "cin%d" % (i % 4))
        sync_group([stg.all()], "cin%d" % (i % 4))
        rb = ring[i % 4][:, 0:nk, 0:ncols]
        k.cp(("dve", "act", "dve", "act", "pool")[i % 5], rb, stg[:, 0:nk, 0:ncols])
        dst = scr[nm]
        P.add("sp", lambda e: e.dma_start(out=dst, in_=rb.ap), reads=[rb], writes=[nm], dma="cout%d" % (i % 4))

    for l in range(L):
        wi = rows_kc(w_in_d[l])
        cast_unit("inT_%d" % l, 8, 328, [(0, 0, wi[:, :, 1024:1280]), (0, 256, wi[:, :, 2112:2176]),
                                         (0, 320, wi[:, :, 2752:2760])])
        cast_unit("in2_%d" % l, 8, 512, [(0, 0, wi[:, :, 1280:1792])])
        cast_unit("in0_%d" % l, 8, 512, [(0, 0, wi[:, :, 0:512])])
        cast_unit("in1_%d" % l, 8, 512, [(0, 0, wi[:, :, 512:1024])])
        cast_unit("in3_%d" % l, 8, 512, [(0, 0, wi[:, :, 1792:2048]), (0, 256, wi[:, :, 2048:2112]),
                                         (0, 320, wi[:, :, 2048:2112]), (0, 384, wi[:, :, 2688:2752]),
                                         (0, 448, wi[:, :, 2688:2752])])
        cast_unit("in4_%d" % l, 8, 512, [(0, 0, wi[:, :, 2176:2688])])
        cast_unit("in5_%d" % l, 8, 256, [(0, 0, wi[:, :, 2760:3016])])
        cast_unit("mem_%d" % l, 8, 512, [(0, 0, rows_kc(w_mem_d[l]))])
        for half in range(2):
            cast_unit("br%d_%d" % (half, l), 8, 512,
                      [(2 * b_, 0, rows_kc(w_branch_d[l, b_])[:, :, 512 * half:512 * half + 512]) for b_ in range(4)])
        for half in range(2):
            for b_ in range(4):
                cast_unit("g%d_%d_%d" % (b_, half, l), 8, 512,
                          [(0, 0, rows_kc(w_gate_d[l, b_])[:, :, 512 * half:512 * half + 512])])
        for half in range(2):
            cast_unit("o%d_%d" % (half, l), 8, 512, [(0, 0, rows_kc(w_out_d[l])[:, :, 512 * half:512 * half + 512])])
        wgu = rows_kc(w_gu_d[l])
        for i in range(11):
            cast_unit("gu%d_%d" % (i, l), 8, 512, [(0, 0, wgu[:, :, 256 * i:256 * i + 256]),
                                                  (0, 256, wgu[:, :, DFF + 256 * i:DFF + 256 * i + 256])])
        wdn = w_dn_d[l].rearrange("(j p) n -> p j n", p=128)
        for half in range(2):
            for jg in range(3):
                nj = min(8, 22 - 8 * jg)
                cast_unit("d%d_%d_%d" % (jg, half, l), nj, 512,
                          [(0, 0, wdn[:, 8 * jg:8 * jg + nj, 512 * half:512 * half + 512])])

    rslot = [0]

    def load_unit(name, nk=8, ncols=512):
        s_ = rslot[0] % 4
        rslot[0] += 1
        dst = ring[s_][:, 0:nk, 0:ncols]
        src = scr[name][:, 0:nk, 0:ncols]
        import os
        if os.environ.get("KNOLOAD") and name.startswith("d"):
            return ring[s_]
        P.add("sp", lambda e: e.dma_start(out=dst.ap, in_=src), reads=[name], writes=[dst], dma="r%d" % s_)
        return ring[s_]

    def finish_rstd(bank, ncols, div, dst):
        k.act(tmpn[:, 0:ncols], pv_(bank, slice(None), slice(0, ncols)), AF.Sqrt, bias=eps_c, scale=1.0 / div)
        k.recip(dst, tmpn[:, 0:ncols])

    def prenorm(src, dst_bf, gain_grp, l, ncols, rst):
        for c in range(8):
            if c % 2 == 0:
                k.act(dst_bf[:, c, 0:ncols], src[:, c, 0:ncols], AF.Square)
            else:
                k.tt("pool", dst_bf[:, c, 0:ncols], src[:, c, 0:ncols], src[:, c, 0:ncols], ALU.mult)
        for c in range(8):
            k.mm(pv_(7, slice(None), slice(0, ncols)), ones_b.all(), dst_bf[:, c, 0:ncols], start=(c == 0), stop=(c == 7))
        finish_rstd(7, ncols, 1024.0, rst)
        for c in range(8):
            k.stt(dst_bf[:, c, 0:ncols], src[:, c, 0:ncols], pvc(l, gain_grp, c), rst, ALU.mult, ALU.mult)

    def post_evac(bank, fc, l, grp):
        import os
        ne = os.environ.get("KNOEVAC", "")
        if "d" not in ne:
            k.ts("dve", bigf[:, fc, :], pv_(bank), pvc(l, grp, fc), None, ALU.mult)
        if "a" not in ne:
            k.act(uT[:, fc, :], pv_(bank), AF.Square)

    def post_finish():
        for fc in range(8):
            k.mm(pv_(7), ones_b.all(), uT[:, fc, :], start=(fc == 0), stop=(fc == 7))
        finish_rstd(7, TB, 1024.0, rstd.all())
        for fc in range(8):
            k.tt("pool", bigf[:, fc, :], bigf[:, fc, :], rstd.all(), ALU.mult)
            k.tt("pool" if fc % 2 else "dve", hT[:, fc, :], hT[:, fc, :], bigf[:, fc, :], ALU.add)

    def fm_chunk(wbuf, col0, bank=None, rhs=None, ncols=TB):
        b_ = ps(bank)
        for kc in range(8):
            k.mm(pv_(b_, slice(None), slice(0, ncols)), wbuf[:, kc, col0:col0 + 128],
                 (rhs if rhs is not None else uT)[:, kc, 0:ncols], start=(kc == 0), stop=(kc == 7))
        return b_


    for b_ in "abcm":
        if b_ not in en:
            k.memset("pool", yT[b_].all(), 0.0)
    S0 = slice(None)
    CI = (IDX_C := (64 ** -0.5) * (8 ** -0.5))

    def mem_setup(s):
        P.tag = 'memsetup'
        for nb in range(2):
            k.dma("sp", xt[:, nb, :], mem_d[s, 128 * nb:128 * nb + 128, :], "xin")
        sync_group([xt[:, 0:2, :]], "xin")
        for c in range(8):
            b_ = ps()
            for nb in range(2):
                k.tr(pv_(b_, S0, slice(128 * nb, 128 * nb + 128)), xt[:, nb, 128 * c:128 * c + 128], ident_f)
            k.cp("act" if c % 2 else "dve", memT[:, c, :], pv_(b_, S0, slice(0, NMEM)))
        for l in range(L):
            prenorm(memT, unT, 4, l, NMEM, rstd[:, 0:NMEM])
            w = load_unit("mem_%d" % l)
            for hp in range(2):
                b_ = fm_chunk(w, 128 * hp, rhs=unT, ncols=NMEM)
                k.cp("act", mk2T[l][:, hp, :], pv_(b_, S0, slice(0, NMEM)))
            for nb in range(2):
                b_ = ps()
                for kc in range(8):
                    k.mm(pv_(b_, S0, slice(0, 256)), unT[:, kc, 128 * nb:128 * nb + 128], w[:, kc, 256:512],
                         start=(kc == 0), stop=(kc == 7))
                src3 = View(psb[b_][:, 0:256].rearrange("p (h d) -> p h d", d=64), [("ps", b_)])
                k.cp("dve", mvdup[l][:, nb, :, 0:64], src3)
                k.cp("act", mvdup[l][:, nb, :, 64:128], src3)

    def load_block(s, blk):
        P.tag = 'load'
        for tt in range(4):
            r0 = blk * TB + 128 * tt
            k.dma("sp", xt[:, tt, :], x_d[s, r0:r0 + 128, :], "xin")
        sync_group([xt.all()], "xin")
        for c in range(8):
            b_ = ps()
            for tt in range(4):
                k.tr(pv_(b_, S0, slice(128 * tt, 128 * tt + 128)), xt[:, tt, 128 * c:128 * c + 128], ident_f)
            k.cp("act" if c % 2 else "dve", hT[:, c, :], pv_(b_))

    outkeys = []

    def store_block(s, blk):
        P.tag = 'store'
        for tt in range(4):
            b0, b1 = ps(), ps()
            for c in range(8):
                bb = b0 if c < 4 else b1
                k.tr(pv_(bb, S0, slice(128 * (c % 4), 128 * (c % 4) + 128)), hT[:, c, 128 * tt:128 * tt + 128], ident_f)
            k.cp("dve", xt[:, tt, 0:512], pv_(b0))
            k.cp("act", xt[:, tt, 512:1024], pv_(b1))
            r0 = blk * TB + 128 * tt
            key = "out%d" % len(outkeys)
            outkeys.append(key)
            k.dma("sp", out_d[s, r0:r0 + 128, :], xt[:, tt, :], "xout%d" % tt, writes=[key])

    def mix(s, blk, l):
        first = (blk == 0)
        W = 16 + TB
        lo_, up_ = slice(0, 64), slice(64, 128)
        P.tag = 'mix_norm'
        prenorm(hT, uT, 0, l, TB, rstd.all())
        P.tag = 'tok'
        wT = load_unit("inT_%d" % l, 8, 328)
        for tt in range(4):
            g = blk * 4 + tt
            b_ = ps()
            for kc in range(8):
                k.mm(pv_(b_, S0, slice(0, 328)), uT[:, kc, 128 * tt:128 * tt + 128], wT[:, kc, 0:328],
                     start=(kc == 0), stop=(kc == 7))
            k.cp("act", itok[:, tt, :], pv_(b_, S0, slice(0, 256)))
            for par in range(2):
                src3 = View(psb[b_][:, 0:256].rearrange("p (hp q d) -> p hp q d", q=2, d=64)[:, :, par, :],
                            [("ps", b_)])
                k.cp("dve", ipad[:, tt, slice(par, 4, 2), 64 * par:64 * par + 64], src3)
            k.cp("dve", vdup[l][:, g, 0:64], pv_(b_, S0, slice(256, 320)))
            k.cp("act", vdup[l][:, g, 64:128], pv_(b_, S0, slice(256, 320)))
            k.act(absw[:, tt, :], pv_(b_, S0, slice(320, 328)), AF.Abs, scale=IDX_C)
            k.act(sgn[:, tt, :], pv_(b_, S0, slice(320, 328)), AF.Sign)
        P.tag = 'gz'
        w2 = load_unit("in2_%d" % l)
        for hp in range(2):
            b_ = fm_chunk(w2, 128 * hp)
            k.act(gs[:, hp, :], pv_(b_), AF.Silu)
        for ci in range(2):
            if first:
                k.memset("pool", zbuf[:, ci, 0:16], 0.0)
            else:
                k.cp("pool", zbuf[:, ci, 0:16], halo[:, l, ci, :])
            b_ = fm_chunk(w2, 256 + 128 * ci)
            k.cp("act", zbuf[:, ci, 16:W], pv_(b_))
            k.cp("pool", halo[:, l, ci, :], zbuf[:, ci, TB:W])
        P.tag = 'hgrn'
        w0 = load_unit("in0_%d" % l)
        w1 = load_unit("in1_%d" % l)
        for h in range(4):
            if "a" not in en:
                break
            hh = h % 2
            bq = fm_chunk(w0, 128 * h)
            bf = fm_chunk(w1, 128 * h)
            lb, oml, noml = lbt[:, l, 0, h:h + 1], lbt[:, l, 1, h:h + 1], lbt[:, l, 2, h:h + 1]
            k.act(hA.all(), pv_(bf), AF.Sigmoid)
            k.act(hC.all(), hA.all(), AF.Identity, bias=oml, scale=noml)
            k.ts("dve", hA.all(), hA.all(), oml, lb, ALU.mult, ALU.add)
            k.act(hA.all(), hA.all(), AF.Ln)
            k.scan(hB.all(), c_("scanmask", 512), hA.all(), 0.0, ALU.mult, ALU.add)
            hB3 = View(hB.h[:, :].rearrange("p (c t) -> p c t", t=64), hB.all().keys)
            k.tt("pool", View(hA2.h[:, :].rearrange("p (c t) -> p c t", t=64), hA2.all().keys), hB3,
                 View(hB.h[:, 63:512:64].unsqueeze(2).broadcast_to([128, 8, 64]), hB.all().keys), ALU.subtract)
            k.act(hE2.all(), hA2.all(), AF.Exp, scale=-1.0)
            k.tt("dve", hF.all(), hC.all(), hE2.all(), ALU.mult)
            k.act(hE.all(), hB.all(), AF.Exp)
            k.cp("dve", dcol.all(), View(hE.h[:, 63:512:64], hE.all().keys))
            bt = ps()
            for p_ in range(4):
                k.tr(pv_(bt, S0, slice(128 * p_, 128 * p_ + 128)), hF[:, 128 * p_:128 * p_ + 128], ident_f)
            src3 = psb[bt][:, :].rearrange("p (a q) -> p a q", q=128)
            k.cp("dve", ktok[lo_, :, 0, :], View(src3[0:64], [("ps", bt)]))
            k.cp("act", ktok[up_, :, 1, :], View(src3[64:128], [("ps", bt)]))
            bu = ps()
            for c in range(8):
                k.mm(pv_(bu, S0, slice(64 * c, 64 * c + 64)), ktok[:, c // 2, c % 2, :],
                     itok[:, c // 2, 64 * h:64 * h + 64])
            S = Sst[:, l, h, :]
            if first:
                k.memset("dve", S, 0.0)
            k.cp("pool", Spad[hh][:, 0, 64 * hh:64 * hh + 64], S)
            for c in range(8):
                src_s = S if c == 0 else Sall[:, c, :]
                dst_s = S if c == 7 else Sall[:, c + 1, :]
                k.stt(dst_s, src_s, dcol[:, c:c + 1], pv_(bu, S0, slice(64 * c, 64 * c + 64)), ALU.mult, ALU.add)
            k.cp("act", Spad[hh][:, 1:8, 64 * hh:64 * hh + 64], Sall[:, 1:8, :])
            k.act(hQ.all(), pv_(bq), AF.Silu)
            k.tt("pool", qS[hh].all(), hQ.all(), hE.all(), ALU.mult)
            k.tt("pool", View(hA.h[:, :].rearrange("p (c t) -> p c t", t=64), hA.all().keys), hB3,
                 View(hB.h[:, 31:512:64].unsqueeze(2).broadcast_to([128, 8, 64]), hB.all().keys), ALU.subtract)
            k.act(hE2.all(), hA.all(), AF.Exp)
            k.tt("dve", Qp.all(), hQ.all(), hE2.all(), ALU.mult)
            k.act(hE.all(), hA.all(), AF.Exp, scale=-1.0)
            k.tt("pool", Kp.all(), hC.all(), hE.all(), ALU.mult)
            ba = ps()
            for p_ in range(4):
                rg = slice(128 * p_, 128 * p_ + 128)
                k.mm(pv_(ba, S0, rg), Kp[:, rg], Qp[:, rg])
            k.tt("dve", attm[hh].all(), pv_(ba), c_("trimask", 512), ALU.mult)
            if hh == 1:
                hp = h // 2
                for p_ in range(4):
                    rg = slice(128 * p_, 128 * p_ + 128)
                    k.mm(pv_(6, S0, rg), ipad[:, p_, 2 * hp, :], attm[0][:, rg], start=True, stop=False)
                    k.mm(pv_(6, S0, rg), ipad[:, p_, 2 * hp + 1, :], attm[1][:, rg], start=False, stop=False)
                    for c in (2 * p_, 2 * p_ + 1):
                        for q in range(2):
                            cs = slice(64 * c, 64 * c + 64)
                            k.mm(pv_(6, S0, cs), Spad[q][:, c, :], qS[q][:, cs], start=False,
                                 stop=(c == 2 * p_ + 1 and q == 1))
                sq = sqr[0]
                k.act(sq.all(), pv_(6), AF.Square)
                bn = ps()
                k.mm(pv_(bn), bdones_b.all(), sq.all())
                k.act(tmpn.all(), pv_(bn), AF.Sqrt, bias=eps_c, scale=1.0 / 64)
                k.recip(oT.all(), tmpn.all())
                k.stt(oT.all(), pv_(6), pv_hgn(l, hp), oT.all(), ALU.mult, ALU.mult)
                k.tt("pool", yT["a"][:, hp, :], oT.all(), gs[:, hp, :], ALU.mult)
        P.tag = 'pool'
        if "b" in en:
            for ci in range(2):
                if ci == 0:
                    k.tt("pool", sfin[lo_, 1:W], zbuf[lo_, ci, 1:W], zbuf[lo_, ci, 0:W - 1], ALU.add)
                    k.tt("pool", s2b[up_, 1:W], zbuf[up_, ci, 1:W], zbuf[up_, ci, 0:W - 1], ALU.add)
                    k.tt("pool", sfin[up_, 3:W], s2b[up_, 3:W], s2b[up_, 1:W - 2], ALU.add)
                else:
                    k.tt("pool", s2b[:, 1:W], zbuf[:, ci, 1:W], zbuf[:, ci, 0:W - 1], ALU.add)
                    k.tt("pool", s4b[:, 3:W], s2b[:, 3:W], s2b[:, 1:W - 2], ALU.add)
                    k.tt("pool", sfin[lo_, 7:W], s4b[lo_, 7:W], s4b[lo_, 3:W - 4], ALU.add)
                    k.tt("pool", s8b[up_, 7:W], s4b[up_, 7:W], s4b[up_, 3:W - 4], ALU.add)
                    k.tt("pool", sfin[up_, 15:W], s8b[up_, 15:W], s8b[up_, 7:W - 8], ALU.add)
                o_ = C_OFF["invw"] + ci
                k.stt(pooled_b[:, ci, :], sfin[:, 16:W], cst[:, o_:o_ + 1], zbuf[:, ci, 16:W], ALU.mult, ALU.subtract)
                if first:
                    o2 = C_OFF["invcnt"] + 16 * ci
                    k.tt("dve", sfin[:, 0:16], sfin[:, 16:32], cst[:, o2:o2 + 16], ALU.mult)
                    k.tt("dve", pooled_b[:, ci, 0:16], sfin[:, 0:16], zbuf[:, ci, 16:32], ALU.subtract)
                b_ = ps()
                k.mm(pv_(b_), poolw_b[:, l, ci, :], pooled_b[:, ci, :])
                k.act(yT["b"][:, ci, :], pv_(b_), AF.Identity, scale=pv_psc(l, ci))
        P.tag = 'dsa_in'
        w3 = load_unit("in3_%d" % l)
        for hp in range(2):
            b_ = fm_chunk(w3, 128 * hp)
            k.act(sqpad[lo_, 2 * hp, :], pv_(b_, lo_), AF.Copy, scale=0.125)
            k.act(sqpad[up_, 2 * hp + 1, :], pv_(b_, up_), AF.Copy, scale=0.125)
        cb = slice(blk * TB, blk * TB + TB)
        b_ = fm_chunk(w3, 256)
        k.cp("dve", skdup[l][:, cb], pv_(b_))
        b_ = fm_chunk(w3, 384)
        k.cp("act", ikdup[l][:, cb], pv_(b_))
        w4 = load_unit("in4_%d" % l)
        for hp in range(4):
            b_ = fm_chunk(w4, 128 * hp)
            k.cp("dve", iqpad[lo_, 2 * hp, :], pv_(b_, lo_))
            k.cp("act", iqpad[up_, 2 * hp + 1, :], pv_(b_, up_))
        w5 = load_unit("in5_%d" % l, 8, 256)
        for hp in range(2):
            b_ = fm_chunk(w5, 128 * hp)
            k.act(mqpad[lo_, 2 * hp, :], pv_(b_, lo_), AF.Copy, scale=0.125)
            k.act(mqpad[up_, 2 * hp + 1, :], pv_(b_, up_), AF.Copy, scale=0.125)
        P.tag = 'dsa'
        def mem_attn():
            P.tag = 'mem'
            if "m" not in en:
                return
            for h in range(4):
                hh, hp = h % 2, h // 2
                for nb in range(2):
                    b_ = ps()
                    k.mm(pv_(b_), mk2T[l][:, hp, 128 * nb:128 * nb + 128], mqpad[:, h, :])
                    p_t = pT[nb]
                    k.act(p_t.all(), pv_(b_), AF.Exp)
                    k.mm(pv_(6), mvdup[l][:, nb, h, :], p_t.all(), start=(nb == 0), stop=(nb == 1))
                for nb in range(2):
                    k.mm(pv_(7), ones_b.all(), pT[nb].all(), start=(nb == 0), stop=(nb == 1))
                rows = lo_ if hh == 0 else up_
                k.recip(rec[rows, :], pv_(7, rows))
                k.tt("dve", yT["m"][rows, hp, :], pv_(6, rows), rec[rows, :], ALU.mult)
            P.tag = 'dsa'

        if "c" in en:
            dsa_score(l, blk, 0)
            dsa_bisect(l, blk, 0)
            mem_attn()
            for tt in range(1, 4):
                dsa_score(l, blk, tt)
                dsa_bisect(l, blk, tt)
                dsa_attend(l, blk, tt - 1)
            dsa_attend(l, blk, 3)
        else:
            mem_attn()
        P.tag = 'mem'
        P.tag = 'gates'
        for half in range(2):
            src = scr["br%d_%d" % (half, l)]
            P.add("sp", (lambda src=src: (lambda e: e.dma_start(out=wbr.all().ap, in_=src)))(),
                  reads=["br%d_%d" % (half, l)], writes=[wbr.all()], dma="wbr")
            for bi, b_name in enumerate("abcm"):
                wg = load_unit("g%d_%d_%d" % (bi, half, l))
                for fq in range(4):
                    fc = 4 * half + fq
                    bg = fm_chunk(wg, 128 * fq)
                    sg_ = sig[fq % 2]
                    k.act(sg_.all(), pv_(bg), AF.Sigmoid)
                    bb = ps()
                    for kc in range(2):
                        k.mm(pv_(bb), wbr[:, 2 * bi + kc, 128 * fq:128 * fq + 128], yT[b_name][:, kc, :],
                             start=(kc == 0), stop=(kc == 1))
                    if bi == 0:
                        k.tt("dve", bigf[:, fc, :], pv_(bb), sg_.all(), ALU.mult)
                    else:
                        t_ = tb[fq % 2]
                        k.tt("dve", t_.all(), pv_(bb), sg_.all(), ALU.mult)
                        if bi < 3:
                            k.tt("pool", bigf[:, fc, :], bigf[:, fc, :], t_.all(), ALU.add)
                        else:
                            k.tt("pool", merged_b[:, fc, :], bigf[:, fc, :], t_.all(), ALU.add)
        P.tag = 'wout'
        for half in range(2):
            wo = load_unit("o%d_%d" % (half, l))
            for fq in range(4):
                fc = 4 * half + fq
                b_ = fm_chunk(wo, 128 * fq, rhs=merged_b)
                post_evac(b_, fc, l, 1)
        post_finish()

    def dsa_score(l, blk, tt):
        g = blk * 4 + tt
        par = g % 2
        N = (g + 1) * 128
        qs = slice(128 * tt, 128 * tt + 128)
        sc = score[par]
        if g < 2:
            return
        for h in range(8):
            k.ts("dve", Dh[:, h, :], ident_b.all(), sgn[:, tt, h:h + 1], None, ALU.mult)
        npiece = (N + 511) // 512
        items = [(j, h) for j in range(npiece) for h in range(8)]
        bl_of = {}

        def geo(j):
            w_ = min(512, N - 512 * j)
            return slice(512 * j, 512 * j + w_), slice(0, w_)

        def logits(i):
            j, h = items[i]
            ks_, ws_ = geo(j)
            bl_of[i] = ps()
            k.mm(pv_(bl_of[i], S0, ws_), iqpad[:, h, qs], ikdup[l][:, ks_])

        LA = 3
        for i in range(min(LA, len(items))):
            logits(i)
        for i, (j, h) in enumerate(items):
            ks_, ws_ = geo(j)
            bsc = 6 + (j % 2)
            r_ = rh[i % 3]
            k.act(r_[:, ws_], pv_(bl_of[i], S0, ws_), AF.Relu, scale=absw[:, tt, h:h + 1])
            if i + LA < len(items):
                logits(i + LA)
            k.mm(pv_(bsc, S0, ws_), Dh[:, h, :], r_[:, ws_], start=(h == 0), stop=(h == 7))
            if h == 7:
                k.cp("act", sc[:, ks_], pv_(bsc, S0, ws_))

    def dsa_bisect(l, blk, tt):
        g = blk * 4 + tt
        par = g % 2
        N = (g + 1) * 128
        sc, mb = score[par], mbias[par]
        if g < 2:
            if N > 128:
                k.memset("pool", mb[:, 0:N - 128], 0.0)
            k.cp("dve", mb[:, N - 128:N], c_("mbdiag", 128))
            return
        A_, lo_c, mid_c, cnt_c, inc_c = [cols[:, par, i:i + 1] for i in range(5)]
        hw = cols[:, par, 8:8 + NBIS]
        k.reduce(A_, sc[:, 0:N], ALU.max, absval=True)
        k.tt("pool", sc[:, N - 128:N], sc[:, N - 128:N], c_("negadm", 128), ALU.add)
        k.ts("dve", hw, c_("pow2", NBIS), A_, None, ALU.mult)
        k.memset("dve", mid_c, 0.0)
        for it in range(NBIS):
            s_i = cols[:, par, 8 + it:9 + it]
            k.ts("dve", mb[:, 0:N], sc[:, 0:N], mid_c, None, ALU.is_ge, ALU.add, accum=cnt_c)
            k.ts("dve", inc_c, cnt_c, 256.0, 0.5, ALU.is_ge, ALU.subtract)
            if it < NBIS - 1:
                k.stt(mid_c, inc_c, s_i, mid_c, ALU.mult, ALU.add)
            else:
                k.ts("dve", inc_c, inc_c, 0.5, None, ALU.subtract)
                k.stt(lo_c, inc_c, s_i, mid_c, ALU.mult, ALU.add)
        k.ts("dve", mb[:, 0:N], sc[:, 0:N], lo_c, -30000.0, ALU.is_lt, ALU.mult)

    def dsa_attend(l, blk, tt):
        g = blk * 4 + tt
        par = g % 2
        qs = slice(128 * tt, 128 * tt + 128)
        lo_, up_ = slice(0, 64), slice(64, 128)
        mb = mbias[par]
        qk_bank = {}

        def qk(kb):
            ksl = slice(128 * kb, 128 * kb + 128)
            b_ = ps()
            qk_bank[kb] = b_
            for h in range(4):
                hs = slice(128 * h, 128 * h + 128)
                k.mm(pv_(b_, S0, hs), mb[:, ksl], ident_b.all(), start=True, stop=False)
                k.mm(pv_(b_, S0, hs), skdup[l][:, ksl], sqpad[:, h, qs], start=False, stop=True)

        qk(0)
        for kb in range(g + 1):
            p_t = pT[kb % 2]
            k.act(p_t.all(), pv_(qk_bank[kb]), AF.Exp)
            if kb + 1 <= g:
                qk(kb + 1)
            k.mm(pv_(6), vdup[l][:, kb, :], p_t.all(), start=(kb == 0), stop=(kb == g))
            k.mm(pv_(7), ones_b.all(), p_t.all(), start=(kb == 0), stop=(kb == g))
        k.recip(rec.all(), pv_(7))
        for h in range(4):
            rows = lo_ if h % 2 == 0 else up_
            hs = slice(128 * h, 128 * h + 128)
            k.tt("dve", yT["c"][rows, h // 2, qs], pv_(6, rows, hs), rec[rows, hs], ALU.mult)

    def ffn(l):
        import os
        kf = int(os.environ.get("KF", "9"))
        P.tag = 'ffn_norm'
        prenorm(hT, uT, 2, l, TB, rstd.all())
        P.tag = 'ffn_gu'
        if kf < 2:
            return
        for i in range(11):
            w = load_unit("gu%d_%d" % (i, l))
            for q in range(2):
                j = 2 * i + q
                bg = fm_chunk(w, 128 * q)
                bu = fm_chunk(w, 256 + 128 * q)
                s_ = sgt[j % 2]
                k.act(s_.all(), pv_(bg), AF.Silu)
                k.tt("dve", actT[:, j, :], pv_(bu), s_.all(), ALU.mult)
        if kf < 3:
            return
        P.tag = 'ffn_down'
        for half in range(2):
            wd = [load_unit("d%d_%d_%d" % (jg, half, l), min(8, 22 - 8 * jg), 512) for jg in range(3)]
            for fq in range(4):
                fc = 4 * half + fq
                b_ = ps()
                for j in range(22):
                    k.mm(pv_(b_), wd[j // 8][:, j % 8, 128 * fq:128 * fq + 128], actT[:, j, :],
                         start=(j == 0), stop=(j == 21))
                post_evac(b_, fc, l, 3)
        if kf < 4:
            return
        post_finish()

    for s in range(nseq):
        if "m" in en:
            mem_setup(s)
        for blk in range(NBLK):
            load_block(s, blk)
            import os
            dbg = os.environ.get("KDBG", "mf")
            for l in range(L):
                if "m" in dbg:
                    mix(s, blk, l)
                if "f" in dbg:
                    ffn(l)
            store_block(s, blk)
    P.add("sp", lambda e: e.nop(), reads=outkeys)
    P.emit()
    return nc, P


_CACHE = {}


def run(inputs, nseq, T, L, ncores, en=("a", "b", "c", "m"), trace=False):
    key = (nseq, T, L, tuple(en))
    if key not in _CACHE:
        _CACHE[key] = build(nseq, T, L, en)[0]
    nc = _CACHE[key]
    f = lambda a: np.ascontiguousarray(np.asarray(a), dtype=np.float32)
    x = f(inputs["x"])
    mem = f(inputs["mem"])
    shared = {
        "consts": CONSTS,
        "pvec": _mk_pvec(inputs, L),
        "poolw": _mk_poolw(f(inputs["pool_w"]), L),
        "w_in": f(inputs["w_in"])[:L],
        "w_gate": f(inputs["w_gate"])[:L],
        "w_branch": f(inputs["w_branch"])[:L],
        "w_out": f(inputs["w_out"])[:L],
        "w_mem_kv": f(inputs["w_mem_kv"])[:L],
        "w_ffn_gate_up": f(inputs["w_ffn_gate_up"])[:L],
        "w_ffn_down": f(inputs["w_ffn_down"])[:L],
    }
    in_maps = []
    for c in range(ncores):
        m = dict(shared)
        m["x"] = np.ascontiguousarray(x[c * nseq:(c + 1) * nseq, :T])
        m["mem"] = np.ascontiguousarray(mem[c * nseq:(c + 1) * nseq])
        in_maps.append(m)
    res = run_bass_kernel_spmd(nc, in_maps, core_ids=list(range(ncores)), trace=trace)
    out = np.concatenate([np.asarray(r["out"]) for r in res.results], axis=0)
    return out.astype(np.float32), res


def kernel(**inputs):
    out, _ = run(inputs, 2, 2048, 2, 8)
    return out
```
